# Optimizing a Trainium2 kernel written in Bass

```python
import math
import jax, jax.numpy as jnp
from jax import lax
import numpy as np

D_MODEL = 1024
BATCH = 4
SEQ = 8192
DEPTH = 1

GRID_W = 64
CTX_LEN = 256
EPS = 1e-6
ROPE_THETA = 10000.0
Q_BLOCK = 128

DIFF_HEADS = 4
DIFF_HEAD_DIM = 64
DIFF_V_DIM = 2 * DIFF_HEAD_DIM
DIFF_WIDTH = DIFF_HEADS * DIFF_V_DIM
DIFF_SCALE = DIFF_HEAD_DIM ** -0.5

MLA_HEADS = 4
MLA_NOPE = 128
MLA_ROPE = 64
MLA_V = 128
MLA_Q_RANK = 256
MLA_KV_RANK = 128
MLA_WIDTH = MLA_HEADS * MLA_V
MLA_SCALE = (MLA_NOPE + MLA_ROPE) ** -0.5

MIX_WIDTH = DIFF_WIDTH + MLA_WIDTH
ROPE_PAIRS = DIFF_HEAD_DIM // 4

IN_SPLITS = (DIFF_HEADS * 2 * DIFF_HEAD_DIM,
             DIFF_HEADS * 2 * DIFF_HEAD_DIM,
             DIFF_HEADS * DIFF_V_DIM,
             MLA_Q_RANK,
             MLA_KV_RANK,
             MLA_ROPE)
IN_COLS = sum(IN_SPLITS)

N_EXPERTS = 32
TOP_K = 4
D_FF = 1024
SWIGLU_LIMIT = 7.0
SWIGLU_ALPHA = 1.702
MOE_BLOCK = 128

kernel_name = "hybrid_diffattn_mla_moe_dit_layer"


def rmsnorm(x, w):
    xf = x.astype(jnp.float32)
    y = xf * lax.rsqrt(jnp.mean(xf * xf, axis=-1, keepdims=True) + EPS)
    return (y * w.astype(jnp.float32)).astype(x.dtype)


def modulate(h, shift, scale):
    return h * (1 + scale) + shift


def axial_angles(n):
    rows = n // GRID_W
    row = jnp.repeat(jnp.arange(rows, dtype=jnp.float32), GRID_W)
    col = jnp.tile(jnp.arange(GRID_W, dtype=jnp.float32), rows)
    inv = ROPE_THETA ** (-jnp.arange(ROPE_PAIRS, dtype=jnp.float32) / ROPE_PAIRS)
    return jnp.stack([row[:, None] * inv, col[:, None] * inv], axis=1)


def apply_axial_rope(x, ang):
    p = x.shape[-1] // 4
    xf = x.astype(jnp.float32).reshape(*x.shape[:-1], 2, 2, p)
    x1, x2 = xf[..., 0, :], xf[..., 1, :]
    cos, sin = jnp.cos(ang), jnp.sin(ang)
    out = jnp.stack([x1 * cos - x2 * sin, x2 * cos + x1 * sin], axis=-2)
    return out.reshape(x.shape).astype(x.dtype)


def mixer_inputs(h, w_in, q_norm_w, kv_norm_w, w_uq, w_ukv, ang):
    B, n, _ = h.shape
    proj = h @ w_in
    cuts = list(np.cumsum(IN_SPLITS)[:-1])
    dq, dk, dv, cq, ckv, kr = jnp.split(proj, cuts, axis=-1)
    dq = dq.reshape(B, n, DIFF_HEADS, 2, DIFF_HEAD_DIM)
    dk = dk.reshape(B, n, DIFF_HEADS, 2, DIFF_HEAD_DIM)
    dv = dv.reshape(B, n, DIFF_HEADS, DIFF_V_DIM)
    q = (rmsnorm(cq, q_norm_w) @ w_uq).reshape(B, n, MLA_HEADS, MLA_NOPE + MLA_ROPE)
    kv = (rmsnorm(ckv, kv_norm_w) @ w_ukv).reshape(B, n, MLA_HEADS, MLA_NOPE + MLA_V)
    q_nope, q_rope = q[..., :MLA_NOPE], q[..., MLA_NOPE:]
    k_nope, mv = kv[..., :MLA_NOPE], kv[..., MLA_NOPE:]
    if ang is not None:
        dq = apply_axial_rope(dq, ang[:, None, None])
        dk = apply_axial_rope(dk, ang[:, None, None])
        q_rope = apply_axial_rope(q_rope, ang[:, None])
        kr = apply_axial_rope(kr, ang)
    return (dq, dk, dv, q_nope, q_rope, k_nope, kr, mv)


def diff_attend(q, k, v, lam):
    s = jnp.einsum('bqhcd,bkhcd->bchqk', q, k).astype(jnp.float32) * DIFF_SCALE
    p = jax.nn.softmax(s, axis=-1)
    p = p[:, 0] - lam * p[:, 1]
    return jnp.einsum('bhqk,bkhe->bqhe', p.astype(v.dtype), v)


def mla_attend(q_nope, q_rope, k_nope, k_rope, v):
    s = (jnp.einsum('bqhd,bkhd->bhqk', q_nope, k_nope)
         + jnp.einsum('bqhr,bkr->bhqk', q_rope, k_rope)).astype(jnp.float32) * MLA_SCALE
    p = jax.nn.softmax(s, axis=-1)
    return jnp.einsum('bhqk,bkhe->bqhe', p.astype(v.dtype), v)


def to_blocks(t):
    B, n = t.shape[:2]
    return jnp.moveaxis(t.reshape(B, n // Q_BLOCK, Q_BLOCK, *t.shape[2:]), 1, 0)


def from_blocks(t):
    t = jnp.moveaxis(t, 0, 1)
    return t.reshape(t.shape[0], -1, *t.shape[3:])


def merge_heads(o_diff, o_mla, subln_w, lam_init, w_out):
    B, n = o_diff.shape[:2]
    o_diff = rmsnorm(o_diff, subln_w) * (1.0 - lam_init)
    mix = jnp.concatenate([o_diff.reshape(B, n, DIFF_WIDTH), o_mla.reshape(B, n, MLA_WIDTH)], axis=-1)
    return mix @ w_out


def clamped_swiglu(g, u):
    g = jnp.minimum(g, SWIGLU_LIMIT)
    u = jnp.clip(u, -SWIGLU_LIMIT, SWIGLU_LIMIT)
    return g * jax.nn.sigmoid(SWIGLU_ALPHA * g) * (u + 1)


def moe(h, router_w, router_b, w1, b1, w2, b2):
    T, D = h.shape
    logits = (h @ router_w + router_b).astype(jnp.float32)
    top_v, top_i = lax.top_k(logits, TOP_K)
    gates = jax.nn.softmax(top_v, axis=-1).astype(h.dtype)
    flat_e = top_i.reshape(-1)
    flat_tok = jnp.repeat(jnp.arange(T, dtype=jnp.int32), TOP_K)
    order = jnp.argsort(flat_e)
    e_sorted = flat_e[order]
    tok_sorted = flat_tok[order]
    gate_sorted = gates.reshape(-1)[order]
    counts = jnp.bincount(flat_e, length=N_EXPERTS)
    padded = ((counts + MOE_BLOCK - 1) // MOE_BLOCK) * MOE_BLOCK
    ends = jnp.cumsum(padded)
    pad_start = ends - padded
    grp_start = jnp.cumsum(counts) - counts
    n_assign = T * TOP_K
    rank = jnp.arange(n_assign, dtype=jnp.int32) - grp_start[e_sorted]
    dest = pad_start[e_sorted] + rank
    n_blocks = (n_assign + N_EXPERTS * (MOE_BLOCK - 1) + MOE_BLOCK - 1) // MOE_BLOCK
    row_tok = jnp.full((n_blocks * MOE_BLOCK,), T, jnp.int32).at[dest].set(tok_sorted)
    h_pad = jnp.concatenate([h, jnp.zeros((1, D), h.dtype)], axis=0)
    xs = h_pad[row_tok].reshape(n_blocks, MOE_BLOCK, D)
    blk_e = jnp.minimum(jnp.searchsorted(ends, jnp.arange(n_blocks) * MOE_BLOCK, side='right'),
                        N_EXPERTS - 1)

    def expert_block(args):
        xb, e = args
        hid = xb @ w1[e] + b1[e]
        return clamped_swiglu(hid[:, :D_FF], hid[:, D_FF:]) @ w2[e] + b2[e]

    ys = lax.map(expert_block, (xs, blk_e)).reshape(-1, D)
    contrib = ys[dest] * gate_sorted[:, None]
    return jax.ops.segment_sum(contrib, tok_sorted, num_segments=T)


def setup_inputs(seed: int = 0) -> dict:
    key = jax.random.key(seed)
    ks = jax.random.split(key, 32)
    f32 = jnp.float32
    L = DEPTH

    def nrm(k, shape, scale):
        return jax.random.normal(k, shape, f32) * scale

    def gain(k, shape):
        return 1.0 + 0.05 * jax.random.normal(k, shape, f32)

    return {
        'x': nrm(ks[0], (BATCH, SEQ, D_MODEL), 1.0),
        'c': nrm(ks[1], (BATCH, D_MODEL), 1.0),
        'ctx': nrm(ks[2], (BATCH, CTX_LEN, D_MODEL), 1.0),
        'c_ctx': nrm(ks[3], (D_MODEL,), 1.0),
        'w_ada': nrm(ks[4], (L, D_MODEL, 6 * D_MODEL), 0.5 * D_MODEL ** -0.5),
        'b_ada': nrm(ks[5], (L, 6 * D_MODEL), 0.02),
        'attn_norm_w': gain(ks[6], (L, D_MODEL)),
        'w_in': nrm(ks[7], (L, D_MODEL, IN_COLS), D_MODEL ** -0.5),
        'q_norm_w': gain(ks[8], (L, MLA_Q_RANK)),
        'kv_norm_w': gain(ks[9], (L, MLA_KV_RANK)),
        'w_uq': nrm(ks[10], (L, MLA_Q_RANK, MLA_HEADS * (MLA_NOPE + MLA_ROPE)), MLA_Q_RANK ** -0.5),
        'w_ukv': nrm(ks[11], (L, MLA_KV_RANK, MLA_HEADS * (MLA_NOPE + MLA_V)), MLA_KV_RANK ** -0.5),
        'lambda_q1': nrm(ks[12], (L, DIFF_HEAD_DIM), 0.1),
        'lambda_k1': nrm(ks[13], (L, DIFF_HEAD_DIM), 0.1),
        'lambda_q2': nrm(ks[14], (L, DIFF_HEAD_DIM), 0.1),
        'lambda_k2': nrm(ks[15], (L, DIFF_HEAD_DIM), 0.1),
        'subln_w': gain(ks[16], (L, DIFF_V_DIM)),
        'w_out': nrm(ks[17], (L, MIX_WIDTH, D_MODEL), MIX_WIDTH ** -0.5),
        'ffn_norm_w': gain(ks[18], (L, D_MODEL)),
        'router_w': nrm(ks[19], (L, D_MODEL, N_EXPERTS), D_MODEL ** -0.5),
        'router_b': nrm(ks[20], (L, N_EXPERTS), 0.01),
        'w1': nrm(ks[21], (L, N_EXPERTS, D_MODEL, 2 * D_FF), D_MODEL ** -0.5),
        'b1': nrm(ks[22], (L, N_EXPERTS, 2 * D_FF), 0.02),
        'w2': nrm(ks[23], (L, N_EXPERTS, D_FF, D_MODEL), D_FF ** -0.5),
        'b2': nrm(ks[24], (L, N_EXPERTS, D_MODEL), 0.02),
        'final_norm_w': gain(ks[25], (D_MODEL,)),
    }


def reference(x, c, ctx, c_ctx, w_ada, b_ada, attn_norm_w, w_in, q_norm_w, kv_norm_w, w_uq, w_ukv,
              lambda_q1, lambda_k1, lambda_q2, lambda_k2, subln_w, w_out, ffn_norm_w,
              router_w, router_b, w1, b1, w2, b2, final_norm_w):
    B, n, D = x.shape
    ang = axial_angles(n)
    for l in range(DEPTH):
        last = l == DEPTH - 1
        lam_init = 0.8 - 0.6 * math.exp(-0.3 * l)
        mod_lat = (jax.nn.silu(c) @ w_ada[l] + b_ada[l])[:, None, :]
        mod_ctx = (jax.nn.silu(c_ctx) @ w_ada[l] + b_ada[l])[None, None, :]
        sa, sca, ga, sf, scf, gf = jnp.split(mod_lat, 6, axis=-1)
        csa, csca, cga, csf, cscf, cgf = jnp.split(mod_ctx, 6, axis=-1)
        lam = (jnp.exp(jnp.sum(lambda_q1[l].astype(jnp.float32) * lambda_k1[l].astype(jnp.float32)))
               - jnp.exp(jnp.sum(lambda_q2[l].astype(jnp.float32) * lambda_k2[l].astype(jnp.float32)))
               + lam_init)
        mp = (w_in[l], q_norm_w[l], kv_norm_w[l], w_uq[l], w_ukv[l])

        l_dq, l_dk, l_dv, l_qn, l_qr, l_kn, l_kr, l_v = mixer_inputs(
            modulate(rmsnorm(x, attn_norm_w[l]), sa, sca), *mp, ang)
        c_dq, c_dk, c_dv, c_qn, c_qr, c_kn, c_kr, c_v = mixer_inputs(
            modulate(rmsnorm(ctx, attn_norm_w[l]), csa, csca), *mp, None)
        dk_all = jnp.concatenate([c_dk, l_dk], axis=1)
        dv_all = jnp.concatenate([c_dv, l_dv], axis=1)
        kn_all = jnp.concatenate([c_kn, l_kn], axis=1)
        kr_all = jnp.concatenate([c_kr, l_kr], axis=1)
        v_all = jnp.concatenate([c_v, l_v], axis=1)
        o_diff = from_blocks(lax.map(lambda qb: diff_attend(qb, dk_all, dv_all, lam), to_blocks(l_dq)))
        o_mla = from_blocks(lax.map(lambda qs: mla_attend(qs[0], qs[1], kn_all, kr_all, v_all),
                                    (to_blocks(l_qn), to_blocks(l_qr))))
        x_new = x + ga * merge_heads(o_diff, o_mla, subln_w[l], lam_init, w_out[l])
        if not last:
            oc_diff = diff_attend(c_dq, c_dk, c_dv, lam)
            oc_mla = mla_attend(c_qn, c_qr, c_kn, c_kr, c_v)
            ctx = ctx + cga * merge_heads(oc_diff, oc_mla, subln_w[l], lam_init, w_out[l])
        x = x_new

        hf = modulate(rmsnorm(x, ffn_norm_w[l]), sf, scf)
        x = x + gf * moe(hf.reshape(B * n, D), router_w[l], router_b[l], w1[l], b1[l], w2[l], b2[l]).reshape(B, n, D)
        if not last:
            hc = modulate(rmsnorm(ctx, ffn_norm_w[l]), csf, cscf)
            ctx = ctx + cgf * moe(hc.reshape(-1, D), router_w[l], router_b[l], w1[l], b1[l], w2[l], b2[l]).reshape(ctx.shape)
    return rmsnorm(x, final_norm_w)
```

```python
import os
import contextlib
import numpy as np
import concourse.bass as bass
import concourse.mybir as mybir
from concourse.bass_utils import run_bass_kernel_spmd

F32 = mybir.dt.float32
BF16 = mybir.dt.bfloat16
ALU = mybir.AluOpType
AF = mybir.ActivationFunctionType

NTOK = 4096
NKEY = 8448
NKT = 66
EPS = 1e-6
DIFF_SCALE = 64 ** -0.5
MLA_SCALE = 192 ** -0.5
LAM_INIT = 0.2
NE = 32
WCOLS = 3072
SPARSE = True
NB = 64
BR = 512
I32 = mybir.dt.int32


class Op:
    __slots__ = ("eng", "fn", "sem", "inc", "waits", "signals", "val")

    def __init__(self, eng, fn, sem, inc):
        self.eng, self.fn, self.sem, self.inc = eng, fn, sem, inc
        self.waits = []
        self.signals = inc == 16
        self.val = None


class Prog:
    def __init__(self, sem_alloc):
        self.ops = []
        self.sem_alloc = sem_alloc
        self.eng_sem = {}
        self.last_w = {}
        self.readers = {}
        self.phase_id = 0
        self.tail = {}

    def _engsem(self, eng):
        if eng not in self.eng_sem:
            self.eng_sem[eng] = self.sem_alloc("p%d_%s" % (self.phase_id, eng))
        return self.eng_sem[eng]

    def op(self, eng, fn, reads=(), writes=(), dma_sem=None):
        if dma_sem is not None:
            o = Op(eng, fn, dma_sem, 16)
            self.tail[id(dma_sem)] = o
        else:
            o = Op(eng, fn, self._engsem(eng), 1)
            self.tail[eng] = o
        deps = []
        for k in reads:
            w = self.last_w.get(k)
            if w is not None:
                deps.append(w)
        for k in writes:
            w = self.last_w.get(k)
            if w is not None:
                deps.append(w)
            deps.extend(self.readers.get(k, ()))
        seen = set()
        for d in deps:
            if d is o or id(d) in seen:
                continue
            seen.add(id(d))
            if d.eng == "pe" and eng == "pe" and d.inc == 1 and dma_sem is None:
                continue
            d.signals = True
            o.waits.append(d)
        for k in reads:
            self.readers.setdefault(k, []).append(o)
        for k in writes:
            self.last_w[k] = o
            self.readers[k] = []
        self.ops.append(o)
        return o

    def barrier(self):
        tails = list(self.tail.values())
        for t in tails:
            t.signals = True
        for eng in ("sync", "act", "dve", "pool", "pe"):
            o = Op(eng, None, None, 0)
            o.signals = False
            o.waits = list(tails)
            self.ops.append(o)
        self.tail = {}
        self.last_w = {}
        self.readers = {}
        self.phase_id += 1
        self.eng_sem = {}

    def finalize(self):
        counts = {}
        for o in self.ops:
            if o.signals:
                key = id(o.sem)
                counts[key] = counts.get(key, 0) + o.inc
                o.val = counts[key]

    def emit(self, name, e):
        waited = {}
        for o in self.ops:
            if o.eng != name:
                continue
            for d in o.waits:
                key = id(d.sem)
                if waited.get(key, 0) >= d.val:
                    continue
                waited[key] = d.val
                e.wait_ge(d.sem, d.val)
            if o.fn is None:
                continue
            ins = o.fn(e)
            if o.signals:
                ins.then_inc(o.sem, o.inc)


def MM(out, pairs):
    def fn(e):
        n = len(pairs)
        ins = None
        for i, (l, r) in enumerate(pairs):
            ins = e.matmul(out, lhsT=l, rhs=r, start=(i == 0), stop=(i == n - 1))
        return ins
    return fn


def MM1(out, l, r, start, stop):
    return lambda e: e.matmul(out, lhsT=l, rhs=r, start=start, stop=stop)


def TRS(out, ins_list, ident):
    def fn(e):
        ins = None
        for o, i in zip(out, ins_list):
            ins = e.transpose(out=o, in_=i, identity=ident)
        return ins
    return fn


def ACT(out, in_, func, **kw):
    return lambda e: e.activation(out=out, in_=in_, func=func, **kw)


def TS(out, in0, s1, s2, op0, op1=None):
    if op1 is None:
        return lambda e: e.tensor_scalar(out=out, in0=in0, scalar1=s1, scalar2=None, op0=op0)
    return lambda e: e.tensor_scalar(out=out, in0=in0, scalar1=s1, scalar2=s2, op0=op0, op1=op1)


def TT(out, in0, in1, op):
    return lambda e: e.tensor_tensor(out=out, in0=in0, in1=in1, op=op)


def STT(out, in0, scalar, in1, op0, op1):
    return lambda e: e.scalar_tensor_tensor(out=out, in0=in0, scalar=scalar, in1=in1, op0=op0, op1=op1)


def CP(out, in_):
    return lambda e: e.tensor_copy(out=out, in_=in_)


def RCP(out, in_):
    return lambda e: e.reciprocal(out=out, in_=in_)


def DMA(out, in_):
    return lambda e: e.dma_start(out=out, in_=in_)


def MEMSET(ap, v):
    return lambda e: e.memset(ap, v)


def build_nc(stop_phase=99, debug=False):
    nc = bass.Bass("TRN2", target_bir_lowering=False)

    def din(name, shape, dt=F32):
        return nc.dram_tensor(name, list(shape), dt, kind="ExternalInput").ap()

    def dscr(name, shape, dt):
        return nc.dram_tensor(name, list(shape), dt, kind=("ExternalOutput" if debug else "Internal")).ap()

    xo = din("xo", [NTOK, 1024])
    xk = din("xk", [NTOK, 1024])
    ctx_d = din("ctx", [256, 1024])
    cvec_d = din("cvec", [128, 16])
    wada_d = din("w_ada", [1024, 6144])
    cols_d = din("cols", [128, 68])
    brow_d = din("brow", [128, 1024 + 32 + 256])
    ident_d = din("ident", [128, 128])
    b1c_d = din("b1c", [128, 512])
    onehot_d = din("onehot", [32, 4096])
    b2_d = din("b2", [32, 1024])
    winx_d = din("w_in_x", [1024, WCOLS])
    wuq_d = din("w_uq_x", [256, 1024])
    wukv_d = din("w_ukv_x", [128, 1024])
    wout_d = din("w_out", [1024, 1024])
    rw_d = din("router_w", [1024, 32])
    if stop_phase >= 4:
        w1_d = din("w1a", [NE, 8, 128, 8, 256])
        w2_d = din("w2", [NE, 1024, 1024])
    kc_d = din("kc", [128, 256])
    b1_d = din("b1", [NE * 16, 128])
    HTOK = dscr("HTOK", [NTOK, 1024], BF16)
    XS = nc.dram_tensor("XS", [NB * BR, 1024], BF16, kind="Internal").ap()
    YS = nc.dram_tensor("YS", [NB * BR, 1024], BF16, kind="Internal").ap()
    ropec_d = din("ropeC", [128, 8192])
    ropes_d = din("ropeS", [128, 8192])
    out_d = nc.dram_tensor("out", [NTOK, 1024], F32, kind="ExternalOutput").ap()

    QD = dscr("QD", [4, 128, NTOK], BF16)
    KD = dscr("KD", [4, 128, NKEY], BF16)
    VD = dscr("VD", [4, 128, NKT, 128], BF16)
    QN = dscr("QN", [4, 128, NTOK], BF16)
    QR = dscr("QR", [4, 64, NTOK], BF16)
    KN = dscr("KN", [4, 128, NKEY], BF16)
    KR = dscr("KR", [64, NKEY], BF16)
    VM = dscr("VM", [4, 128, NKT, 128], BF16)
    XN = dscr("XN", [NTOK, 1024], F32)
    HF = dscr("HF", [8, 128, NTOK], BF16)
    dbg_mod = dscr("dbg_mod", [128, 96], F32) if debug else None
    dbg_mix = dscr("dbg_mix", [8, 128, NTOK], BF16) if debug else None
    dbg_G = dscr("dbg_G", [128, 32 * 32], F32) if debug else None
    if debug:
        dbg_dest = dscr("dbg_dest", [128, 128], F32)
        dbg_gate = dscr("dbg_gate", [128, 128], F32)
        dbg_blk = dscr("dbg_blk", [128, NB], F32)
        dbg_md = dscr("dbg_md", [32, 8], F32)
        dbg_xs = dscr("dbg_xs", [1024, 1024], BF16)
        dbg_ys = dscr("dbg_ys", [1024, 1024], F32)

    es = contextlib.ExitStack()
    with es:
        sems = []

        def sem_alloc(name):
            s = es.enter_context(nc.semaphore(name))
            sems.append(s)
            return s

        P = Prog(sem_alloc)
        dsem_pool = []
        dsem_map = {}

        def dsem(name):
            if name not in dsem_map:
                i = len(dsem_map)
                if i >= len(dsem_pool):
                    dsem_pool.append(sem_alloc("dq%d" % i))
                dsem_map[name] = dsem_pool[i]
            return dsem_map[name]

        _orig_barrier = P.barrier

        def _barrier():
            _orig_barrier()
            dsem_map.clear()
        P.barrier = _barrier

        SB_LO, SB_HI = 16512, 229344
        cur = [SB_LO]
        names = [0]
        limit = [SB_HI]
        LATE_BASE = SB_HI - 16384

        def sb(shape, dt):
            nbytes = int(np.prod(shape[1:])) * (2 if dt == BF16 else 4)
            off = (cur[0] + 63) // 64 * 64
            assert off + nbytes <= limit[0], ("SBUF overflow", off, nbytes)
            cur[0] = off + nbytes
            names[0] += 1
            return nc.alloc_sbuf_tensor_at("t%d" % names[0], list(shape), dt, offset=off)

        pb = [nc.alloc_psum_tensor("pb%d" % i, [128, 512], F32) for i in range(8)]
        pbb = [p.bitcast(BF16) for p in pb]

        def PK(i):
            return ("ps", i)

        ident32 = sb([128, 128], F32)
        identb = sb([128, 128], BF16)
        ones32 = sb([128, 128], F32)
        onesb = sb([128, 128], BF16)
        cols = sb([128, 68], F32)
        brow = sb([128, 1312], F32)
        modT = sb([128, 48, 2], F32)
        gw = sb([128, 8], F32)
        gcw = sb([128, 8], F32)
        gfw = sb([128, 8], F32)
        gf_bc = sb([128, 1024], F32)
        b1c = sb([128, 512], F32)
        G = sb([128, 32, 32], F32)
        onehot = sb([32, 4096], BF16)
        b2b = sb([32, 1024], BF16)
        lamc = sb([128, 8], F32)
        subw = sb([128, 1], F32)
        epsc = sb([128, 1], F32)
        kc = sb([128, 256], F32)
        PERSIST_END = cur[0]
        cur[0] = LATE_BASE
        Mall = sb([128, 32, 32], F32)
        LG = sb([128, 32, 32], F32)
        TOP = sb([128, 32, 8], F32)
        DESTI = sb([128, 4, 32], I32)
        GATE = sb([128, 4, 32], F32)
        WIDXI = sb([128, NB, 8], I32)
        B1IDXI = sb([16, NB], I32)
        B2IDXI = sb([2, NB], I32)
        assert cur[0] <= SB_HI
        cur[0] = PERSIST_END
        wuqb = sb([128, 2, 1024], BF16)
        wukvb = sb([128, 1024], BF16)

        sa_col = lambda k: modT[:, 0 + k, 0:1]
        csa_col = lambda k: modT[:, 0 + k, 1:2]
        sf_col = lambda k: modT[:, 24 + k, 0:1]
        fnw_bc = brow[:, 0:1024]
        rb_bc = brow[:, 1024:1056]

        cv = sb([128, 16], F32)
        scv = sb([128, 16], F32)
        sgv = sb([128, 16], F32)
        wad = [sb([128, 8, 1024], F32) for _ in range(2)]
        lt = sb([128, 128], F32)
        diag = sb([128, 128], F32)
        wuq32 = sb([128, 2, 1024], F32)
        wukv32 = sb([128, 1024], F32)
        P.op("sync", DMA(wuq32[:], wuq_d.rearrange("(k p) n -> p k n", p=128)), writes=["wuq32"], dma_sem=dsem("wuq32"))
        P.op("sync", DMA(wukv32[:], wukv_d), writes=["wukv32"], dma_sem=dsem("wukv32"))

        P.op("sync", DMA(cv[:], cvec_d), writes=["cv"], dma_sem=dsem("cv"))
        P.op("sync", DMA(cols[:], cols_d), writes=["cols"], dma_sem=dsem("cols"))
        P.op("sync", DMA(brow[:], brow_d), writes=["brow"], dma_sem=dsem("brow"))
        P.op("sync", DMA(ident32[:], ident_d), writes=["ident32"], dma_sem=dsem("ident"))
        P.op("sync", DMA(b1c[:], b1c_d), writes=["b1c"], dma_sem=dsem("b1c"))
        P.op("sync", DMA(kc[:], kc_d), writes=["kc"], dma_sem=dsem("kc"))
        P.op("pool", DMA(onehot[:], onehot_d), writes=["onehot"], dma_sem=dsem("onehot"))
        P.op("pool", DMA(b2b[:], b2_d), writes=["b2b"], dma_sem=dsem("b2b"))
        P.op("dve", CP(identb[:], ident32[:]), reads=["ident32"], writes=["identb"])
        P.op("dve", MEMSET(ones32[:], 1.0), writes=["ones32"])
        P.op("dve", MEMSET(onesb[:], 1.0), writes=["onesb"])
        P.op("dve", MEMSET(epsc[:], EPS), writes=["epsc"])
        P.op("act", ACT(sgv[:], cv[:], AF.Sigmoid), reads=["cv"], writes=["sgv"])
        P.op("dve", TT(scv[:], cv[:], sgv[:], ALU.mult), reads=["cv", "sgv"], writes=["scv"])
        for m in range(6):
            s = m % 2
            src = wada_d[:, m * 1024:(m + 1) * 1024].rearrange("(k p) n -> p k n", p=128)
            P.op("sync", DMA(wad[s][:, 0:4, :], src[:, 0:4, :]), writes=[("wad", s, 0)], dma_sem=dsem("wad%d_0" % s))
            P.op("sync", DMA(wad[s][:, 4:8, :], src[:, 4:8, :]), writes=[("wad", s, 1)], dma_sem=dsem("wad%d_1" % s))
            bank = m % 2
            for fc in range(8):
                pairs = [(wad[s][:, k, fc * 128:(fc + 1) * 128], scv[:, 2 * k:2 * k + 2]) for k in range(8)]
                P.op("pe", MM(pb[bank][:, 2 * fc:2 * fc + 2], pairs),
                     reads=[("wad", s, 0), ("wad", s, 1), "scv"], writes=[PK(bank)])
            pv = pb[bank][:, 0:16].rearrange("p (a b) -> p a b", b=2)
            for j in range(2):
                P.op("dve", TT(modT[:, m * 8:(m + 1) * 8, j], pv[:, :, j], cols[:, 16 + m * 8:16 + (m + 1) * 8], ALU.add),
                     reads=[PK(bank), "cols"], writes=["modT"])
        P.op("dve", STT(gw[:], modT[:, 8:16, 0], 1.0, cols[:, 0:8], ALU.add, ALU.mult), reads=["modT", "cols"], writes=["gw"])
        P.op("dve", STT(gcw[:], modT[:, 8:16, 1], 1.0, cols[:, 0:8], ALU.add, ALU.mult), reads=["modT", "cols"], writes=["gcw"])
        P.op("dve", STT(gfw[:], modT[:, 32:40, 0], 1.0, cols[:, 8:16], ALU.add, ALU.mult), reads=["modT", "cols"], writes=["gfw"])
        def bcast_gate(mi, dst, nm, colfn=None):
            for k in range(8):
                bank = 2 + (k % 2)
                col = modT[:, mi * 8 + k, 0:1] if colfn is None else colfn(k)
                P.op("dve", TS(diag[:], ident32[:], col, None, ALU.mult),
                     reads=["ident32", "modT"], writes=["diag"])
                P.op("pe", MM(pb[bank][:, 0:128], [(ones32[:], diag[:])]), reads=["ones32", "diag"], writes=[PK(bank)])
                P.op("act", ACT(dst[:, k * 128:(k + 1) * 128], pb[bank][:, 0:128], AF.Identity), reads=[PK(bank)], writes=[nm])
        bcast_gate(5, gf_bc, "gf_bc")
        lv = brow[:, 1056:1312]
        P.op("dve", TT(lt[:, 0:64], lv[:, 0:64], lv[:, 64:128], ALU.mult), reads=["brow"], writes=["lt"])
        P.op("dve", TT(lt[:, 64:128], lv[:, 128:192], lv[:, 192:256], ALU.mult), reads=["brow"], writes=["lt"])
        P.op("dve", lambda e: e.reduce_sum(out=lamc[:, 0:1], in_=lt[:, 0:64], axis=mybir.AxisListType.X), reads=["lt"], writes=["lamc"])
        P.op("dve", lambda e: e.reduce_sum(out=lamc[:, 1:2], in_=lt[:, 64:128], axis=mybir.AxisListType.X), reads=["lt"], writes=["lamc"])
        P.op("act", ACT(lamc[:, 2:4], lamc[:, 0:2], AF.Exp), reads=["lamc"], writes=["lamc"])
        P.op("dve", TT(lamc[:, 4:5], lamc[:, 3:4], lamc[:, 2:3], ALU.subtract), reads=["lamc"], writes=["lamc"])
        P.op("dve", TS(lamc[:, 4:5], lamc[:, 4:5], -LAM_INIT, None, ALU.add), reads=["lamc"], writes=["lamc"])
        P.op("dve", TS(subw[:], cols[:, 67:68], 1.0 - LAM_INIT, None, ALU.mult), reads=["cols"], writes=["subw"])
        for k in range(2):
            P.op("dve", TS(wuqb[:, k, :], wuq32[:, k, :], cols[:, 64 + k:65 + k], None, ALU.mult),
                 reads=["wuq32", "cols"], writes=["wuqb"])
        P.op("dve", TS(wukvb[:], wukv32[:], cols[:, 66:67], None, ALU.mult), reads=["wukv32", "cols"], writes=["wukvb"])
        if debug:
            P.op("sync", DMA(dbg_mod, modT[:].rearrange("p a b -> p (a b)")), reads=["modT"], writes=["dbg_mod"], dma_sem=dsem("dbg_mod"))
        P.barrier()

        def rstd_chain(ss_ap, ms_ap, sq_ap, r_ap, n, key, eng_extra_reads=()):
            P.op("act", ACT(sq_ap, ss_ap, AF.Sqrt, scale=1.0 / n, bias=epsc[0:ss_ap.shape[0], 0:1]),
                 reads=[key + "_ss", "epsc"] + list(eng_extra_reads), writes=[key + "_sq"])
            P.op("dve", RCP(r_ap, sq_ap), reads=[key + "_sq"], writes=[key + "_r"])

        if stop_phase >= 1:
            cur[0] = PERSIST_END
            wuqb = sb([128, 2, 1024], BF16)
            wukvb = sb([128, 1024], BF16)
            Wb = sb([128, 8, WCOLS], BF16)
            xt = [sb([128, 1024], F32) for _ in range(3)]
            junk = sb([128, 1024], BF16)
            xsb = [sb([128, 1024], BF16) for _ in range(2)]
            st = sb([128, 16], F32)
            hT = [sb([128, 8, 512], BF16) for _ in range(2)]
            rC = [sb([128, 512], F32) for _ in range(2)]
            rS = [sb([128, 512], F32) for _ in range(2)]
            t1 = [sb([128, 512], F32) for _ in range(2)]
            t2 = [sb([128, 512], F32) for _ in range(2)]
            t3 = [sb([128, 512], F32)] * 2
            cqT = sb([128, 2, 512], BF16)
            ckvT = sb([128, 512], BF16)
            sq32 = sb([128, 2, 512], F32)
            sqk32 = sb([128, 512], F32)
            rq_sq = sb([128, 512], F32)
            rq_bc = sb([128, 512], F32)
            rk_sq = sb([128, 512], F32)
            rk_bc = sb([128, 512], F32)
            rkc = sb([128, 12], F32)
            o_qd = [sb([128, 4, 512], BF16)] * 2
            o_kd = [sb([128, 4, 512], BF16) for _ in range(2)]
            o_vd = [sb([128, 4, 512], BF16) for _ in range(2)]
            o_qn = [sb([128, 4, 512], BF16)] * 2
            o_qr = [sb([128, 2, 512], BF16)] * 2
            o_kn = [sb([128, 4, 512], BF16) for _ in range(2)]
            o_kr = [sb([64, 512], BF16) for _ in range(2)]
            o_vm = [sb([128, 4, 512], BF16) for _ in range(2)]

            wv = winx_d.rearrange("(k p) n -> p k n", p=128)
            for i in range(4):
                P.op("pool", DMA(Wb[:, 2 * i:2 * i + 2, :], wv[:, 2 * i:2 * i + 2, :]), writes=[("Wb", i)], dma_sem=dsem("Wb%d" % i))
            WbK = [("Wb", i) for i in range(4)]

            groups = [("ctx", 0, 2)] + [("own", g * 512, 4) for g in range(8)] + [("oth", g * 512, 4) for g in range(8)]
            bank_rr = [0]

            def nbank():
                b = 2 + (bank_rr[0] % 6)
                bank_rr[0] += 1
                return b

            tile_ctr = [0]
            def tiles_gen(gi):
                (kind, T0, nt) = groups[gi]
                N = nt * 128
                s = gi % 2
                src = {"ctx": ctx_d, "own": xo, "oth": xk}[kind]
                key0 = {"ctx": 0, "own": 256, "oth": 256 + 4096}[kind] + T0
                rope = kind != "ctx"
                own = kind == "own"
                g_w = gcw if kind == "ctx" else gw
                sac = csa_col if kind == "ctx" else sa_col
                if rope:
                    rcol = T0 + (4096 if kind == "oth" else 0)
                    P.op("sync", DMA(rC[s][:], ropec_d[:, rcol:rcol + 512]), writes=[("rC", s)], dma_sem=dsem("rC%d" % s))
                    P.op("sync", DMA(rS[s][:], ropes_d[:, rcol:rcol + 512]), writes=[("rS", s)], dma_sem=dsem("rS%d" % s))
                for t in range(nt):
                    tc = tile_ctr[0]
                    tile_ctr[0] += 1
                    xs_ = tc % 3
                    ss_ = tc % 4
                    x2 = tc % 2
                    P.op("sync", DMA(xt[xs_][:], src[T0 + t * 128:T0 + (t + 1) * 128, :]), writes=[("xt", xs_)], dma_sem=dsem("xt%d" % xs_))
                    kk = "st%d" % ss_
                    P.op("act", ACT(junk[:], xt[xs_][:], AF.Square, accum_out=st[:, 4 * ss_:4 * ss_ + 1]),
                         reads=[("xt", xs_)], writes=["junk", kk + "_ss"])
                    rstd_chain(st[:, 4 * ss_:4 * ss_ + 1], None, st[:, 4 * ss_ + 1:4 * ss_ + 2], st[:, 4 * ss_ + 2:4 * ss_ + 3], 1024.0, kk)
                    P.op("dve", TS(xsb[x2][:], xt[xs_][:], st[:, 4 * ss_ + 2:4 * ss_ + 3], None, ALU.mult),
                         reads=[("xt", xs_), kk + "_r"], writes=[("xsb", x2)])
                    yield
                    tb = tc % 2
                    P.op("pe", TRS([pbb[tb][:, k * 128:(k + 1) * 128] for k in range(8)],
                                   [xsb[x2][:, k * 128:(k + 1) * 128] for k in range(8)], identb[:]),
                         reads=[("xsb", x2), "identb"], writes=[PK(tb)])
                    for k in range(8):
                        P.op("act", ACT(hT[s][:, k, t * 128:(t + 1) * 128], pbb[tb][:, k * 128:(k + 1) * 128], AF.Identity,
                                        scale=g_w[:, k:k + 1], bias=sac(k)),
                             reads=[PK(tb), "gw", "gcw", "modT"], writes=[("hT", s, t)])
                    yield

            def proj_gen(gi):
                (kind, T0, nt) = groups[gi]
                N = nt * 128
                s = gi % 2
                src = {"ctx": ctx_d, "own": xo, "oth": xk}[kind]
                key0 = {"ctx": 0, "own": 256, "oth": 256 + 4096}[kind] + T0
                rope = kind != "ctx"
                own = kind == "own"
                g_w = gcw if kind == "ctx" else gw
                sac = csa_col if kind == "ctx" else sa_col
                hTk = [("hT", s, t) for t in range(nt)]

                def proj(col0, width=128):
                    b = nbank()
                    pairs = [(Wb[:, k, col0:col0 + width], hT[s][:, k, 0:N]) for k in range(8)]
                    P.op("pe", MM(pb[b][0:width, 0:N], pairs), reads=hTk + WbK, writes=[PK(b)])
                    return b

                def rope_out(bA, bB, dst, rows, dkey, post_scale=None):
                    i = bank_rr[0] % 2
                    P.op("dve", TT(t1[i][0:rows, 0:N], pb[bA][0:rows, 0:N], rC[s][0:rows, 0:N], ALU.mult),
                         reads=[PK(bA), ("rC", s)], writes=[("t1", i)])
                    P.op("dve", TT(t2[i][0:rows, 0:N], pb[bB][0:rows, 0:N], rS[s][0:rows, 0:N], ALU.mult),
                         reads=[PK(bB), ("rS", s)], writes=[("t2", i)])
                    if post_scale is None:
                        P.op("dve", TT(dst, t1[i][0:rows, 0:N], t2[i][0:rows, 0:N], ALU.add),
                             reads=[("t1", i), ("t2", i)], writes=[dkey])
                    else:
                        P.op("dve", TT(t3[i][0:rows, 0:N], t1[i][0:rows, 0:N], t2[i][0:rows, 0:N], ALU.add),
                             reads=[("t1", i), ("t2", i)], writes=[("t3", 0)])
                        P.op("dve", TT(dst, t3[i][0:rows, 0:N], post_scale[0:rows, 0:N], ALU.mult),
                             reads=[("t3", 0), "rq_r"], writes=[dkey])

                if own:
                    for h in range(4):
                        bA = proj(h * 128)
                        bB = proj(512 + h * 128)
                        rope_out(bA, bB, o_qd[s][:, h, 0:N], 128, ("o_qd", 0))
                        yield
                    P.op("sync", DMA(QD[:, :, T0:T0 + N].rearrange("h p t -> p h t"), o_qd[s][:, :, 0:N]),
                         reads=[("o_qd", 0)], writes=["QD"], dma_sem=dsem("o_qd"))
                for h in range(4):
                    bA = proj(1024 + h * 128)
                    if rope:
                        bB = proj(1536 + h * 128)
                        rope_out(bA, bB, o_kd[s][:, h, 0:N], 128, ("o_kd", s))
                        yield
                    else:
                        P.op("act", ACT(o_kd[s][:, h, 0:N], pb[bA][:, 0:N], AF.Identity), reads=[PK(bA)], writes=[("o_kd", s)])
                        yield
                P.op("sync", DMA(KD[:, :, key0:key0 + N].rearrange("h p t -> p h t"), o_kd[s][:, :, 0:N]),
                     reads=[("o_kd", s)], writes=["KD"], dma_sem=dsem("o_kd%d" % s))
                bA = proj(2944, 64)
                if rope:
                    bB = proj(3008, 64)
                    rope_out(bA, bB, o_kr[s][:, 0:N], 64, ("o_kr", s))
                    yield
                else:
                    P.op("act", ACT(o_kr[s][:, 0:N], pb[bA][0:64, 0:N], AF.Identity), reads=[PK(bA)], writes=[("o_kr", s)])
                    yield
                P.op("sync", DMA(KR[:, key0:key0 + N], o_kr[s][:, 0:N]), reads=[("o_kr", s)], writes=["KR"], dma_sem=dsem("o_kr%d" % s))
                for t in range(nt):
                    b = nbank()
                    pairs = [(hT[s][:, k, t * 128:(t + 1) * 128], Wb[:, k, 2048:2560]) for k in range(8)]
                    P.op("pe", MM(pb[b][:, :], pairs), reads=[("hT", s, t)] + WbK, writes=[PK(b)])
                    P.op("act", ACT(o_vd[s][:, t, :], pb[b][:, :], AF.Identity), reads=[PK(b)], writes=[("o_vd", s)])
                    yield
                kt0 = key0 // 128
                for h in range(4):
                    P.op("sync", DMA(VD[h, :, kt0:kt0 + nt, :], o_vd[s][:, 0:nt, h * 128:(h + 1) * 128]),
                         reads=[("o_vd", s)], writes=["VD"], dma_sem=dsem("o_vd%d_%d" % (s, h)))
                b = proj(2816)
                P.op("act", ACT(ckvT[:, 0:N], pb[b][:, 0:N], AF.Identity), reads=[PK(b)], writes=["ckvT"])
                P.op("act", ACT(sqk32[:, 0:N], pb[b][:, 0:N], AF.Square), reads=[PK(b)], writes=["sqk32"])
                b = nbank()
                P.op("pe", MM(pb[b][:, 0:N], [(ones32[:], sqk32[:, 0:N])]), reads=["ones32", "sqk32"], writes=[PK(b), "rk_ss"])
                rstd_chain(pb[b][:, 0:N], None, rk_sq[:, 0:N], rk_bc[:, 0:N], 128.0, "rk", eng_extra_reads=[PK(b)])
                b = nbank()
                for t in range(nt):
                    P.op("pe", MM(pb[b][:, t:t + 1], [(sqk32[:, t * 128:(t + 1) * 128], ones32[:, 0:1])]),
                         reads=["sqk32", "ones32"], writes=[PK(b), "rkc_ss"])
                rstd_chain(pb[b][:, 0:nt], None, rkc[:, 0:nt], rkc[:, 4:4 + nt], 128.0, "rkc", eng_extra_reads=[PK(b)])
                for h in range(4):
                    b = nbank()
                    P.op("pe", MM(pb[b][:, 0:N], [(wukvb[:, h * 128:(h + 1) * 128], ckvT[:, 0:N])]), reads=["wukvb", "ckvT"], writes=[PK(b)])
                    P.op("dve", TT(o_kn[s][:, h, 0:N], pb[b][:, 0:N], rk_bc[:, 0:N], ALU.mult), reads=[PK(b), "rk_r"], writes=[("o_kn", s)])
                    yield
                P.op("sync", DMA(KN[:, :, key0:key0 + N].rearrange("h p t -> p h t"), o_kn[s][:, :, 0:N]),
                     reads=[("o_kn", s)], writes=["KN"], dma_sem=dsem("o_kn%d" % s))
                for t in range(nt):
                    b = nbank()
                    P.op("pe", MM(pb[b][:, :], [(ckvT[:, t * 128:(t + 1) * 128], wukvb[:, 512:1024])]), reads=["wukvb", "ckvT"], writes=[PK(b)])
                    P.op("act", ACT(o_vm[s][:, t, :], pb[b][:, :], AF.Identity, scale=rkc[:, 4 + t:5 + t]), reads=[PK(b), "rkc_r"], writes=[("o_vm", s)])
                    yield
                for h in range(4):
                    P.op("sync", DMA(VM[h, :, kt0:kt0 + nt, :], o_vm[s][:, 0:nt, h * 128:(h + 1) * 128]),
                         reads=[("o_vm", s)], writes=["VM"], dma_sem=dsem("o_vm%d_%d" % (s, h)))
                if own:
                    for c in range(2):
                        b = proj(2560 + c * 128)
                        P.op("act", ACT(cqT[:, c, :], pb[b][:, 0:N], AF.Identity), reads=[PK(b)], writes=["cqT"])
                        P.op("act", ACT(sq32[:, c, :], pb[b][:, 0:N], AF.Square), reads=[PK(b)], writes=["sq32"])
                    b = nbank()
                    P.op("pe", MM(pb[b][:, 0:N], [(ones32[:], sq32[:, 0, :]), (ones32[:], sq32[:, 1, :])]),
                         reads=["ones32", "sq32"], writes=[PK(b), "rq_ss"])
                    rstd_chain(pb[b][:, 0:N], None, rq_sq[:, 0:N], rq_bc[:, 0:N], 256.0, "rq", eng_extra_reads=[PK(b)])
                    for h in range(4):
                        b = nbank()
                        P.op("pe", MM(pb[b][:, 0:N], [(wuqb[:, k, h * 128:(h + 1) * 128], cqT[:, k, :]) for k in range(2)]),
                             reads=["wuqb", "cqT"], writes=[PK(b)])
                        P.op("dve", TT(o_qn[s][:, h, 0:N], pb[b][:, 0:N], rq_bc[:, 0:N], ALU.mult), reads=[PK(b), "rq_r"], writes=[("o_qn", 0)])
                        yield
                    P.op("sync", DMA(QN[:, :, T0:T0 + N].rearrange("h p t -> p h t"), o_qn[s][:, :, 0:N]),
                         reads=[("o_qn", 0)], writes=["QN"], dma_sem=dsem("o_qn"))
                    for r in range(2):
                        bA = nbank()
                        P.op("pe", MM(pb[bA][:, 0:N], [(wuqb[:, k, 512 + r * 128:512 + (r + 1) * 128], cqT[:, k, :]) for k in range(2)]),
                             reads=["wuqb", "cqT"], writes=[PK(bA)])
                        bB = nbank()
                        P.op("pe", MM(pb[bB][:, 0:N], [(wuqb[:, k, 768 + r * 128:768 + (r + 1) * 128], cqT[:, k, :]) for k in range(2)]),
                             reads=["wuqb", "cqT"], writes=[PK(bB)])
                        rope_out(bA, bB, o_qr[s][:, r, 0:N], 128, ("o_qr", 0), post_scale=rq_bc)
                        yield
                    for r in range(2):
                        for hh in range(2):
                            P.op("sync", DMA(QR[2 * r + hh, :, T0:T0 + N], o_qr[s][hh * 64:(hh + 1) * 64, r, 0:N]),
                                 reads=[("o_qr", 0)], writes=["QR"], dma_sem=dsem("o_qr_%d" % (2 * r + hh)))

            for _ in tiles_gen(0):
                pass
            for gi in range(len(groups)):
                tg = tiles_gen(gi + 1) if gi + 1 < len(groups) else iter(())
                step = 0
                for _ in proj_gen(gi):
                    step += 1
                    if step % 2 == 0:
                        next(tg, None)
                for _ in tg:
                    pass
            P.barrier()

        cur[0] = PERSIST_END
        mixT = sb([128, 8, NTOK], BF16)
        PH2_END = cur[0]
        if stop_phase >= 2:
            Kt = [sb([128, NKEY], BF16) for _ in range(2)]
            Vt = [sb([128, NKT, 128], BF16) for _ in range(2)]
            KRt = sb([128, NKEY], BF16)
            Qa = [sb([128, 512], BF16) for _ in range(2)]
            Qb = [sb([128, 512], BF16) for _ in range(2)]
            Pt = [sb([128, 512], BF16) for _ in range(6)]
            e0 = sb([128, 512], F32)
            e1 = sb([128, 512], F32)
            e2 = sb([128, 512], F32)
            e3 = sb([128, 512], F32)
            e4 = sb([128, 512], F32)
            e5 = sb([128, 512], F32)
            P.op("sync", DMA(KRt[0:64, :], KR), writes=["KRt"], dma_sem=dsem("KRt"))
            P.op("dve", MEMSET(KRt[64:128, :], 0.0), writes=["KRtz"])
            for i in range(2):
                P.op("dve", MEMSET(Qa[i][:], 0.0), writes=[("Qa", i)])
                P.op("dve", MEMSET(Qb[i][:], 0.0), writes=[("Qb", i)])
            pctr = [0]
            qctr = [0]

            def load_head(hh):
                s = hh % 2
                if hh < 4:
                    ksrc, vsrc = KD[hh], VD[hh]
                else:
                    ksrc, vsrc = KN[hh - 4], VM[hh - 4]
                P.op("sync", DMA(Kt[s][:, 0:4224], ksrc[:, 0:4224]), writes=[("Kt", s, 0)], dma_sem=dsem("Kt%d_0" % s))
                P.op("sync", DMA(Kt[s][:, 4224:NKEY], ksrc[:, 4224:NKEY]), writes=[("Kt", s, 1)], dma_sem=dsem("Kt%d_1" % s))
                P.op("sync", DMA(Vt[s][:, 0:33, :], vsrc[:, 0:33, :]), writes=[("Vt", s, 0)], dma_sem=dsem("Vt%d_0" % s))
                P.op("sync", DMA(Vt[s][:, 33:NKT, :], vsrc[:, 33:NKT, :]), writes=[("Vt", s, 1)], dma_sem=dsem("Vt%d_1" % s))

            racc = sb([128, 512], F32)
            LA = 3
            items = [(hh, qg, kt, c) for hh in range(8) for qg in range(8) for kt in range(NKT) for c in range(2 if hh < 4 else 1)]
            NI = len(items)

            def emit_S(idx):
                hh, qg, kt, c = items[idx]
                s = hh % 2
                diff = hh < 4
                h = hh if diff else hh - 4
                qs = (hh * 8 + qg) % 2
                q0 = qg * 512
                if kt == 0 and c == 0:
                    if diff:
                        P.op("sync", DMA(Qa[qs][0:64, :], QD[h, 0:64, q0:q0 + 512]), writes=[("Qa", qs)], dma_sem=dsem("Qa%d" % qs))
                        P.op("sync", DMA(Qb[qs][64:128, :], QD[h, 64:128, q0:q0 + 512]), writes=[("Qb", qs)], dma_sem=dsem("Qb%d" % qs))
                    else:
                        P.op("sync", DMA(Qa[qs][:, :], QN[h, :, q0:q0 + 512]), writes=[("Qa", qs)], dma_sem=dsem("Qa%d" % qs))
                        P.op("sync", DMA(Qb[qs][0:64, :], QR[h, :, q0:q0 + 512]), writes=[("Qb", qs)], dma_sem=dsem("Qb%d" % qs))
                half = 0 if kt < 33 else 1
                kcols = slice(kt * 128, (kt + 1) * 128)
                sbk = idx % 4
                if diff:
                    qsrc = Qa[qs] if c == 0 else Qb[qs]
                    P.op("pe", MM(pb[sbk][:, :], [(Kt[s][:, kcols], qsrc[:, :])]),
                         reads=[("Kt", s, half), ("Qa", qs), ("Qb", qs)], writes=[PK(sbk)])
                else:
                    P.op("pe", MM(pb[sbk][:, :], [(Kt[s][:, kcols], Qa[qs][:, :]), (KRt[:, kcols], Qb[qs][:, :])]),
                         reads=[("Kt", s, half), ("Qa", qs), ("Qb", qs), "KRt", "KRtz"], writes=[PK(sbk)])

            load_head(0)
            for idx in range(LA):
                emit_S(idx)
            for idx in range(NI):
                hh, qg, kt, c = items[idx]
                if qg == 0 and kt == 0 and c == 0 and hh + 1 < 8:
                    load_head(hh + 1)
                if idx + LA < NI:
                    emit_S(idx + LA)
                s = hh % 2
                diff = hh < 4
                h = hh if diff else hh - 4
                q0 = qg * 512
                half = 0 if kt < 33 else 1
                sbk = idx % 4
                pi = idx % 6
                scale = DIFF_SCALE if diff else MLA_SCALE
                P.op("act", ACT(Pt[pi][:], pb[sbk][:, :], AF.Exp, scale=scale), reads=[PK(sbk)], writes=[("Pt", pi)])
                P.op("pe", MM1(pb[4 + c][:, :], Vt[s][:, kt, :], Pt[pi][:], kt == 0, kt == NKT - 1),
                     reads=[("Vt", s, half), ("Pt", pi)], writes=[PK(4 + c)])
                if c == 1:
                    P.op("pe", MM1(pb[7][:, :], onesb[:], Pt[pi][:], kt == 0, kt == NKT - 1),
                         reads=["onesb", ("Pt", pi)], writes=[PK(7)])
                elif (not diff) and kt % 2 == 1:
                    P.op("pe", MM1(pb[6][:, :], onesb[:], Pt[pi][:], kt == 1, False),
                         reads=["onesb", ("Pt", pi)], writes=[PK(6)])
                else:
                    if kt == 0:
                        P.op("dve", CP(racc[:], Pt[pi][:]), reads=[("Pt", pi)], writes=["racc"])
                    else:
                        P.op("dve", TT(racc[:], racc[:], Pt[pi][:], ALU.add), reads=[("Pt", pi), "racc"], writes=["racc"])
                last = (kt == NKT - 1) and (c == (1 if diff else 0))
                if not last:
                    continue
                P.op("pe", MM1(pb[6][:, :], ones32[:], racc[:], diff, True), reads=["ones32", "racc"], writes=[PK(6)])
                if diff:
                    P.op("dve", RCP(e0[:], pb[6][:, :]), reads=[PK(6)], writes=["e0"])
                    P.op("dve", TT(e1[:], pb[4][:, :], e0[:], ALU.mult), reads=[PK(4), "e0"], writes=["e1"])
                    P.op("dve", RCP(e2[:], pb[7][:, :]), reads=[PK(7)], writes=["e2"])
                    P.op("dve", TT(e3[:], pb[5][:, :], e2[:], ALU.mult), reads=[PK(5), "e2"], writes=["e3"])
                    P.op("dve", STT(e1[:], e3[:], lamc[:, 4:5], e1[:], ALU.mult, ALU.add), reads=["e1", "e3", "lamc"], writes=["e1"])
                    P.op("act", ACT(e0[:], e1[:], AF.Square), reads=["e1"], writes=["e0"])
                    P.op("pe", MM(pb[6][:, :], [(ones32[:], e0[:])]), reads=["ones32", "e0"], writes=[PK(6), "sub_ss"])
                    rstd_chain(pb[6][:, :], None, e4[:], e5[:], 128.0, "sub", eng_extra_reads=[PK(6)])
                    P.op("dve", STT(mixT[:, h, q0:q0 + 512], e1[:], subw[:, 0:1], e5[:], ALU.mult, ALU.mult),
                         reads=["e1", "sub_r", "subw"], writes=[("mixT", hh, qg)])
                else:
                    P.op("dve", RCP(e0[:], pb[6][:, :]), reads=[PK(6)], writes=["e0"])
                    P.op("dve", TT(mixT[:, 4 + h, q0:q0 + 512], pb[4][:, :], e0[:], ALU.mult), reads=[PK(4), "e0"], writes=[("mixT", hh, qg)])
            if debug:
                P.op("sync", DMA(dbg_mix.rearrange("k p t -> p k t"), mixT[:]), reads=[("mixT", a, b_) for a in range(8) for b_ in range(8)],
                     writes=["dbg_mix"], dma_sem=dsem("dbg_mix"))
            P.barrier()

        if stop_phase >= 3:
            limit[0] = LATE_BASE
            cur[0] = PH2_END
            woutb = sb([128, 8, 1024], BF16)
            rw32 = sb([128, 8, 32], F32)
            x3 = [sb([128, 1024], F32) for _ in range(2)]
            y3 = [sb([128, 1024], F32) for _ in range(2)]
            xn3 = [sb([128, 1024], F32) for _ in range(2)]
            xs3 = [sb([128, 1024], F32) for _ in range(2)]
            hf32 = [sb([128, 8, 128], F32) for _ in range(2)]
            hfb = [sb([128, 8, 128], BF16) for _ in range(2)]
            st3 = sb([128, 8], F32)
            gfw_bc = sb([128, 1024], F32)
            sf_bc = sb([128, 1024], F32)
            htk = [sb([128, 1024], BF16) for _ in range(2)]
            ex = [sb([128, 32], F32) for _ in range(2)]
            gs3 = sb([128, 8], F32)
            ga_bc = sb([128, 1024], F32)
            diag = sb([128, 128], F32)
            bcast_gate(2, ga_bc, "ga_bc")
            bcast_gate(0, gfw_bc, "gfw_bc", colfn=lambda k: gfw[:, k:k + 1])
            bcast_gate(0, sf_bc, "sf_bc", colfn=sf_col)
            wo_v = wout_d.rearrange("(k p) n -> p k n", p=128)
            P.op("pool", DMA(woutb[:, 0:4, :], wo_v[:, 0:4, :]), writes=["woutb0"], dma_sem=dsem("woutb0"))
            P.op("pool", DMA(woutb[:, 4:8, :], wo_v[:, 4:8, :]), writes=["woutb1"], dma_sem=dsem("woutb1"))
            P.op("sync", DMA(rw32[:], rw_d.rearrange("(k p) n -> p k n", p=128)), writes=["rw32"], dma_sem=dsem("rw32"))
            for t in range(32):
                s = t % 2
                tk = slice(t * 128, (t + 1) * 128)
                P.op("sync", DMA(x3[s][:], xo[tk, :]), writes=[("x3", s)], dma_sem=dsem("x3_%d" % s))
                for hlf in range(2):
                    bk = hlf + 2 * s
                    P.op("pe", MM(pb[bk][:, :], [(mixT[:, k, tk], woutb[:, k, hlf * 512:(hlf + 1) * 512]) for k in range(8)]),
                         reads=["woutb0", "woutb1"], writes=[PK(bk)])
                    P.op("dve", TT(y3[s][:, hlf * 512:(hlf + 1) * 512], pb[bk][:, :], ga_bc[:, hlf * 512:(hlf + 1) * 512], ALU.mult),
                         reads=[PK(bk)], writes=[("y3", s)])
                P.op("dve", TT(xn3[s][:], y3[s][:], x3[s][:], ALU.add), reads=[("y3", s), ("x3", s)], writes=[("xn3", s)])
                P.op("sync", DMA(XN[tk, :], xn3[s][:]), reads=[("xn3", s)], writes=["XN"], dma_sem=dsem("xn3_%d" % s))
                kk = "s3_%d" % s
                P.op("act", ACT(y3[s][:], xn3[s][:], AF.Square, accum_out=st3[:, 4 * s:4 * s + 1]), reads=[("xn3", s)], writes=[("y3", s), kk + "_ss"])
                rstd_chain(st3[:, 4 * s:4 * s + 1], None, st3[:, 4 * s + 1:4 * s + 2], st3[:, 4 * s + 2:4 * s + 3], 1024.0, kk)
                P.op("dve", TS(xs3[s][:], xn3[s][:], st3[:, 4 * s + 2:4 * s + 3], None, ALU.mult), reads=[("xn3", s), kk + "_r"], writes=[("xs3", s)])
                for hb in range(2):
                    bk = 4 + 2 * s + hb
                    P.op("pe", TRS([pb[bk][:, j * 128:(j + 1) * 128] for j in range(4)],
                                   [xs3[s][:, (4 * hb + j) * 128:(4 * hb + j + 1) * 128] for j in range(4)], ident32[:]),
                         reads=[("xs3", s), "ident32"], writes=[PK(bk)])
                    for j in range(4):
                        k = 4 * hb + j
                        P.op("act", ACT(hf32[s][:, k, :], pb[bk][:, j * 128:(j + 1) * 128], AF.Identity, scale=gfw[:, k:k + 1], bias=sf_col(k)),
                             reads=[PK(bk)], writes=[("hf32", s)])
                if not SPARSE:
                    P.op("pool", CP(hfb[s][:], hf32[s][:]), reads=[("hf32", s)], writes=[("hfb", s)])
                    P.op("sync", DMA(HF[:, :, tk].rearrange("k p t -> p k t"), hfb[s][:]), reads=[("hfb", s)], writes=["HF"], dma_sem=dsem("hfb%d" % s))
                else:
                    P.op("dve", TT(xs3[s][:], xs3[s][:], gfw_bc[:], ALU.mult), reads=[("xs3", s), "gfw_bc"] + [PK(4 + 2 * s), PK(5 + 2 * s)], writes=[("xs3", s)])
                    P.op("dve", TT(htk[s][:], xs3[s][:], sf_bc[:], ALU.add), reads=[("xs3", s), "sf_bc"], writes=[("htk", s)])
                    P.op("sync", DMA(HTOK[tk, :], htk[s][:]), reads=[("htk", s)], writes=["HTOK"], dma_sem=dsem("htk%d" % s))
                bk = s
                lb = 4 + 2 * s
                P.op("pe", MM(pb[lb][:, 0:32], [(hf32[s][:, k, :], rw32[:, k, :]) for k in range(8)]),
                     reads=[("hf32", s), "rw32"], writes=[PK(lb)])
                lg_t, top_t, msk_t = LG[:, t, :], TOP[:, t, :], Mall[:, t, :]
                P.op("dve", TT(lg_t, pb[lb][:, 0:32], rb_bc, ALU.add), reads=[PK(lb)], writes=[("lg", s)])
                P.op("dve", (lambda o_, i_: (lambda e: e.max(out=o_, in_=i_)))(top_t, lg_t), reads=[("lg", s)], writes=[("top8", s)])
                P.op("dve", TS(msk_t, lg_t, TOP[:, t, 3:4], None, ALU.is_ge), reads=[("lg", s), ("top8", s)], writes=[("msk", s)])
                P.op("dve", TS(gs3[:, 4 * s:4 * s + 1], TOP[:, t, 0:1], -1.0, None, ALU.mult), reads=[("top8", s)], writes=[("gs3", s, 0)])
                P.op("act", ACT(ex[s][:], lg_t, AF.Exp, bias=gs3[:, 4 * s:4 * s + 1]), reads=[("lg", s), ("gs3", s, 0)], writes=[("ex", s)])
                P.op("dve", TT(ex[s][:], ex[s][:], msk_t, ALU.mult), reads=[("ex", s), ("msk", s)], writes=[("ex", s)])
                P.op("dve", lambda e, s=s: e.reduce_sum(out=gs3[:, 4 * s + 1:4 * s + 2], in_=ex[s][:], axis=mybir.AxisListType.X),
                     reads=[("ex", s)], writes=[("gs3", s, 1)])
                P.op("dve", RCP(gs3[:, 4 * s + 2:4 * s + 3], gs3[:, 4 * s + 1:4 * s + 2]), reads=[("gs3", s, 1)], writes=[("gs3", s, 2)])
                P.op("dve", TS(G[:, t, :], ex[s][:], gs3[:, 4 * s + 2:4 * s + 3], None, ALU.mult), reads=[("ex", s), ("gs3", s, 2)], writes=["G"])
            if debug:
                P.op("sync", DMA(dbg_G, G[:].rearrange("p a b -> p (a b)")), reads=["G"], writes=["dbg_G"], dma_sem=dsem("dbg_G"))
            P.barrier()

        def IGATHER(out, in_, idx):
            return lambda e: e.indirect_dma_start(out=out, out_offset=None, in_=in_, in_offset=bass.IndirectOffsetOnAxis(ap=idx, axis=0))

        def ISCATTER(out, idx, in_):
            return lambda e: e.indirect_dma_start(out=out, out_offset=bass.IndirectOffsetOnAxis(ap=idx, axis=0), in_=in_, in_offset=None)

        def RSUM(out, in_):
            return lambda e: e.reduce_sum(out=out, in_=in_, axis=mybir.AxisListType.X)

        if stop_phase >= 4 and SPARSE:
            cur[0] = PERSIST_END
            md = sb([32, 8], F32)
            cmp8 = sb([32, 8], F32)
            psbc = sb([32, 128], F32)
            base = sb([128, 32], F32)
            DEST = sb([128, 32, 32], F32)
            sel = sb([128, 32, 32], F32)
            tmp = sb([128, 32, 32], F32)
            DESTF = sb([128, 4, 32], F32)
            cmpj = sb([32, NB], F32)
            blkbc = sb([128, NB], F32)
            WIDXF = sb([128, NB, 8], F32)
            B1F = sb([16, NB], F32)
            htk4 = [sb([128, 1024], BF16) for _ in range(2)]
            P.op("pe", MM(pb[0][0:32, 0:1], [(Mall[:, t, :], ones32[:, 0:1]) for t in range(32)]), reads=[], writes=[PK(0)])
            P.op("dve", CP(md[:, 0:1], pb[0][0:32, 0:1]), reads=[PK(0)], writes=["md0"])
            P.op("dve", TS(cmp8[:], kc[0:32, 160:168], md[:, 0:1], None, ALU.is_lt), reads=["md0"], writes=["cmp8"])
            P.op("dve", RSUM(md[:, 1:2], cmp8[:]), reads=["cmp8"], writes=["md1"])
            P.op("dve", TS(md[:, 2:3], md[:, 1:2], float(BR), None, ALU.mult), reads=["md1"], writes=["md2"])
            P.op("pe", MM(pb[1][0:32, 0:1], [(kc[0:32, 128:160], md[:, 2:3])]), reads=["md2"], writes=[PK(1)])
            P.op("dve", CP(md[:, 3:4], pb[1][0:32, 0:1]), reads=[PK(1)], writes=["md3"])
            P.op("dve", TT(md[:, 4:5], md[:, 3:4], md[:, 2:3], ALU.subtract), reads=["md3", "md2"], writes=["md4"])
            P.op("dve", TS(psbc[:], ones32[0:32, :], md[:, 4:5], None, ALU.mult), reads=["md4"], writes=["psbc"])
            P.op("pe", MM(pb[2][:, 0:32], [(psbc[:], ident32[0:32, 0:32])]), reads=["psbc"], writes=[PK(2)])
            P.op("dve", CP(base[:], pb[2][:, 0:32]), reads=[PK(2)], writes=["base"])
            for i in range(32):
                bk, bk2 = 3 + i % 2, 5 + i % 2
                P.op("pe", MM(pb[bk][:, 0:32], [(kc[:, 0:128], Mall[:, i, :])]), reads=[], writes=[PK(bk)])
                P.op("pe", MM(pb[bk2][:, 0:32], [(ones32[:], Mall[:, i, :])]), reads=[], writes=[PK(bk2)])
                P.op("dve", TT(DEST[:, i, :], pb[bk][:, 0:32], base[:], ALU.add), reads=[PK(bk), "base"], writes=["DEST"])
                P.op("dve", TT(base[:], base[:], pb[bk2][:, 0:32], ALU.add), reads=[PK(bk2), "base"], writes=["base"])
            for k in range(4):
                topk_bc = TOP[:, :, k:k + 1].to_broadcast([128, 32, 32])
                P.op("dve", TT(sel[:], LG[:], topk_bc, ALU.is_equal), reads=[], writes=["sel"])
                P.op("dve", TT(tmp[:], sel[:], DEST[:], ALU.mult), reads=["sel", "DEST"], writes=["tmp"])
                P.op("dve", RSUM(DESTF[:, k, :], tmp[:]), reads=["tmp"], writes=[("DESTF", k)])
                P.op("dve", TT(tmp[:], sel[:], G[:], ALU.mult), reads=["sel"], writes=["tmp"])
                P.op("dve", RSUM(GATE[:, k, :], tmp[:]), reads=["tmp"], writes=[("GATE", k)])
            P.op("dve", TS(DESTF[:], DESTF[:], 0.0, float(NB * BR - 1), ALU.max, ALU.min), reads=[("DESTF", k) for k in range(4)], writes=[("DESTF", k) for k in range(4)])
            P.op("dve", CP(DESTI[:], DESTF[:]), reads=[("DESTF", k) for k in range(4)], writes=["DESTI"])
            P.op("dve", TS(cmpj[:], kc[0:32, 168:168 + NB], md[:, 3:4], None, ALU.is_ge), reads=["md3"], writes=["cmpj"])
            P.op("pe", MM(pb[7][:, 0:NB], [(ones32[0:32, :], cmpj[:])]), reads=["cmpj"], writes=[PK(7)])
            P.op("dve", TS(blkbc[:], pb[7][:, 0:NB], float(NE - 1), None, ALU.min), reads=[PK(7)], writes=["blkbc"])
            for jj in range(8):
                P.op("dve", TS(WIDXF[:, :, jj], blkbc[:], 1024.0, kc[:, 232 + jj:233 + jj], ALU.mult, ALU.add), reads=["blkbc"], writes=["WIDXF"])
            P.op("dve", CP(WIDXI[:], WIDXF[:]), reads=["WIDXF"], writes=["WIDXI"])
            P.op("dve", TS(B1F[:], blkbc[0:16, :], 16.0, kc[0:16, 240:241], ALU.mult, ALU.add), reads=["blkbc"], writes=["B1F"])
            P.op("dve", CP(B1IDXI[:], B1F[:]), reads=["B1F"], writes=["B1IDXI"])
            P.op("dve", CP(B2IDXI[:], blkbc[0:2, :]), reads=["blkbc"], writes=["B2IDXI"])
            if debug:
                P.op("sync", DMA(dbg_dest, DESTF[:].rearrange("p a b -> p (a b)")), reads=[("DESTF", k) for k in range(4)], writes=["dbg_dest"], dma_sem=dsem("dbg1"))
                P.op("sync", DMA(dbg_gate, GATE[:].rearrange("p a b -> p (a b)")), reads=[("GATE", k) for k in range(4)], writes=["dbg_gate"], dma_sem=dsem("dbg2"))
                P.op("sync", DMA(dbg_blk, blkbc[:]), reads=["blkbc"], writes=["dbg_blk"], dma_sem=dsem("dbg3"))
                P.op("sync", DMA(dbg_md, md[:]), reads=["md0", "md1", "md2", "md3", "md4"], writes=["dbg_md"], dma_sem=dsem("dbg4"))
            for i in range(32):
                s = i % 2
                P.op("sync", DMA(htk4[s][:], HTOK[i * 128:(i + 1) * 128, :]), writes=[("htk4", s)], dma_sem=dsem("htk4_%d" % s))
                for k in range(4):
                    P.op("pool", ISCATTER(XS, DESTI[:, k, i:i + 1], htk4[s][:]), reads=[("htk4", s), "DESTI"], writes=[("XSw", s, k)],
                         dma_sem=dsem("sc%d_%d" % (s, k)))
            P.barrier()

            cur[0] = PERSIST_END
            w1b = [sb([128, 2048], BF16) for _ in range(16)]
            w2b = [sb([128, 8, 1024], BF16) for _ in range(2)]
            b1g = [sb([16, 128], F32) for _ in range(2)]
            b2r = [sb([2, 1024], BF16) for _ in range(2)]
            b1col = [sb([128, 16], F32) for _ in range(2)]
            xtok = [sb([128, 4, 1024], BF16) for _ in range(2)]
            xT = sb([128, 8, 512], BF16)
            actT = sb([128, 8, 512], BF16)
            gc = [sb([128, 512], F32) for _ in range(2)]
            sg = [sb([128, 512], F32) for _ in range(2)]
            uc = [sb([128, 512], F32) for _ in range(2)]
            ysb = [sb([128, 1024], BF16) for _ in range(2)]
            w1flat = w1_d.rearrange("e j p k c -> (e j p) (k c)")
            w2flat = w2_d.rearrange("e r n -> (e r) n")

            def issue_block_loads(b):
                ws = b % 2
                P.op("sync", DMA(xtok[ws][:], XS[b * BR:(b + 1) * BR, :].rearrange("(t p) d -> p t d", p=128)), writes=[("xtok", ws)], dma_sem=dsem("xtok%d" % ws))
                for jj in range(8):
                    slot = (b * 8 + jj) % 16
                    P.op("pool", IGATHER(w1b[slot][:], w1flat, WIDXI[:, b, jj:jj + 1]), writes=[("w1b", slot)], dma_sem=dsem("w1b%d" % slot))
                P.op("pool", IGATHER(b1g[ws][:], b1_d, B1IDXI[:, b:b + 1]), writes=[("b1g", ws)], dma_sem=dsem("b1g%d" % ws))
                for jj in range(8):
                    P.op("pool", IGATHER(w2b[ws][:, jj, :], w2flat, WIDXI[:, b, jj:jj + 1]), writes=[("w2b", ws, jj)], dma_sem=dsem("w2b%d_%d" % (ws, jj)))
                P.op("pool", IGATHER(b2r[ws][:], b2_d, B2IDXI[:, b:b + 1]), writes=[("b2r", ws)], dma_sem=dsem("b2r%d" % ws))

            issue_block_loads(0)
            sw = [0]
            yb = [0]
            ys_ctr = [0]
            for b in range(NB):
                ws = b % 2
                if b + 1 < NB:
                    issue_block_loads(b + 1)
                for t in range(4):
                    tb = t % 2
                    P.op("pe", TRS([pbb[tb][:, k * 128:(k + 1) * 128] for k in range(8)],
                                   [xtok[ws][:, t, k * 128:(k + 1) * 128] for k in range(8)], identb[:]),
                         reads=[("xtok", ws)], writes=[PK(tb)])
                    P.op("act", ACT(xT[:, :, t * 128:(t + 1) * 128], pbb[tb][:, :].rearrange("p (k t) -> p k t", k=8), AF.Identity),
                         reads=[PK(tb)], writes=[("xT", t)])
                P.op("pe", TRS([pb[0][:, 0:16]], [b1g[ws][:, :]], ident32[0:16, 0:16]), reads=[("b1g", ws)], writes=[PK(0)])
                P.op("dve", CP(b1col[ws][:], pb[0][:, 0:16]), reads=[PK(0)], writes=[("b1col", ws)])
                xTk = [("xT", t) for t in range(4)]
                for jj in range(8):
                    slot = (b * 8 + jj) % 16
                    i = sw[0] % 2
                    sw[0] += 1
                    bg, bu = 2 + 2 * i, 3 + 2 * i
                    P.op("pe", MM(pb[bg][:, :], [(w1b[slot][:, k * 256:k * 256 + 128], xT[:, k, :]) for k in range(8)]),
                         reads=[("w1b", slot)] + xTk, writes=[PK(bg)])
                    P.op("pe", MM(pb[bu][:, :], [(w1b[slot][:, k * 256 + 128:k * 256 + 256], xT[:, k, :]) for k in range(8)]),
                         reads=[("w1b", slot)] + xTk, writes=[PK(bu)])
                    P.op("dve", TS(gc[i][:], pb[bg][:, :], b1col[ws][:, jj:jj + 1], 7.0, ALU.add, ALU.min), reads=[PK(bg), ("b1col", ws)], writes=[("gc", i)])
                    P.op("act", ACT(sg[i][:], gc[i][:], AF.Sigmoid, scale=1.702), reads=[("gc", i)], writes=[("sg", i)])
                    P.op("dve", TS(uc[i][:], pb[bu][:, :], b1col[ws][:, 8 + jj:9 + jj], 7.0, ALU.add, ALU.min), reads=[PK(bu), ("b1col", ws)], writes=[("uc", i)])
                    P.op("dve", TS(uc[i][:], uc[i][:], -7.0, 1.0, ALU.max, ALU.add), reads=[("uc", i)], writes=[("uc", i)])
                    P.op("dve", TT(gc[i][:], gc[i][:], sg[i][:], ALU.mult), reads=[("gc", i), ("sg", i)], writes=[("gc", i)])
                    P.op("dve", TT(actT[:, jj, :], gc[i][:], uc[i][:], ALU.mult), reads=[("gc", i), ("uc", i)], writes=[("actT", jj)])
                for t in range(4):
                    ys_ = ys_ctr[0] % 2
                    ys_ctr[0] += 1
                    for hlf in range(2):
                        bk = 6 + (yb[0] % 2)
                        yb[0] += 1
                        cs = slice(hlf * 512, (hlf + 1) * 512)
                        pairs = [(actT[:, jj, t * 128:(t + 1) * 128], w2b[ws][:, jj, cs]) for jj in range(8)]
                        pairs.append((onesb[0:1, 0:128], b2r[ws][0:1, cs]))
                        P.op("pe", MM(pb[bk][:, :], pairs), reads=[("actT", jj) for jj in range(8)] + [("w2b", ws, jj) for jj in range(8)] + [("b2r", ws)],
                             writes=[PK(bk)])
                        P.op("act", ACT(ysb[ys_][:, cs], pb[bk][:, :], AF.Identity), reads=[PK(bk)], writes=[("ysb", ys_, hlf)])
                    P.op("sync", DMA(YS[b * BR + t * 128:b * BR + (t + 1) * 128, :], ysb[ys_][:]), reads=[("ysb", ys_, 0), ("ysb", ys_, 1)],
                         writes=["YS"], dma_sem=dsem("ysb%d" % ys_))
            P.barrier()

            if debug:
                P.op("sync", DMA(dbg_xs, XS[0:1024, :]), writes=["dbg_xs"], dma_sem=dsem("dbg5"))
                P.op("sync", DMA(dbg_ys, YS[0:1024, :]), writes=["dbg_ys"], dma_sem=dsem("dbg6"))
                P.barrier()
            cur[0] = PERSIST_END
            yk = [[sb([128, 1024], BF16) for _ in range(4)] for _ in range(2)]
            acc5 = [sb([128, 1024], F32) for _ in range(2)]
            x5 = [sb([128, 1024], F32) for _ in range(2)]
            o5 = [sb([128, 1024], F32) for _ in range(2)]
            st5 = sb([128, 8], F32)

            def issue_gathers(i):
                s = i % 2
                for k in range(4):
                    P.op("pool", IGATHER(yk[s][k][:], YS, DESTI[:, k, i:i + 1]), writes=[("yk", s, k)], dma_sem=dsem("yk%d_%d" % (s, k)))
                P.op("sync", DMA(x5[s][:], XN[i * 128:(i + 1) * 128, :]), writes=[("x5", s)], dma_sem=dsem("x5_%d" % s))

            issue_gathers(0)
            for i in range(32):
                s = i % 2
                if i + 1 < 32:
                    issue_gathers(i + 1)
                P.op("dve", TS(acc5[s][:], yk[s][0][:], GATE[:, 0, i:i + 1], None, ALU.mult), reads=[("yk", s, 0)], writes=[("acc5", s)])
                for k in range(1, 4):
                    P.op("dve", STT(acc5[s][:], yk[s][k][:], GATE[:, k, i:i + 1], acc5[s][:], ALU.mult, ALU.add),
                         reads=[("yk", s, k), ("acc5", s)], writes=[("acc5", s)])
                P.op("dve", TT(o5[s][:], acc5[s][:], gf_bc[:], ALU.mult), reads=[("acc5", s)], writes=[("o5", s)])
                P.op("dve", TT(x5[s][:], x5[s][:], o5[s][:], ALU.add), reads=[("x5", s), ("o5", s)], writes=[("x5", s)])
                kk = "s5_%d" % s
                P.op("act", ACT(o5[s][:], x5[s][:], AF.Square, accum_out=st5[:, 4 * s:4 * s + 1]), reads=[("x5", s)], writes=[("o5", s), kk + "_ss"])
                rstd_chain(st5[:, 4 * s:4 * s + 1], None, st5[:, 4 * s + 1:4 * s + 2], st5[:, 4 * s + 2:4 * s + 3], 1024.0, kk)
                P.op("dve", STT(o5[s][:], x5[s][:], st5[:, 4 * s + 2:4 * s + 3], fnw_bc, ALU.mult, ALU.mult),
                     reads=[("x5", s), kk + "_r", ("o5", s)], writes=[("o5", s)])
                P.op("sync", DMA(out_d[i * 128:(i + 1) * 128, :], o5[s][:]), reads=[("o5", s)], writes=["out"], dma_sem=dsem("o5_%d" % s))
            P.barrier()

        if stop_phase >= 4 and not SPARSE:
            cur[0] = PERSIST_END
            acc2 = sb([128, 16, 1024], F32)
            hfT = sb([128, 8, 2048], BF16)
            off_act = (cur[0] + 63) // 64 * 64
            actT = sb([128, 8, 2048], BF16)
            w1b = [sb([128, 8, 256], BF16) for _ in range(4)]
            w2b = sb([128, 8, 1024], BF16)
            gc = [sb([128, 512], F32) for _ in range(2)]
            sg = [sb([128, 512], F32) for _ in range(2)]
            uc = [sb([128, 512], F32) for _ in range(2)]
            st5 = sb([128, 8], F32)
            end4 = cur[0]
            cur[0] = off_act
            x5 = [sb([128, 1024], F32) for _ in range(2)]
            o5 = [sb([128, 1024], F32) for _ in range(2)]
            cur[0] = end4
            sw = [0]
            yb = [0]
            for hf_i in range(2):
                tok0 = hf_i * 2048
                for k in range(8):
                    P.op("sync", DMA(hfT[:, k, :], HF[k, :, tok0:tok0 + 2048]), writes=["hfT"], dma_sem=dsem("hfT%d" % k))

                def issue_w1(idx):
                    ws_ = idx % 4
                    P.op("pool", DMA(w1b[ws_][:], w1_d[idx // 8, idx % 8]), writes=[("w1b", ws_)], dma_sem=dsem("w1b%d" % ws_))

                def issue_w2(e_):
                    w2v = w2_d[e_].rearrange("(j p) n -> p j n", p=128)
                    P.op("pool", DMA(w2b[:, 0:4, :], w2v[:, 0:4, :]), writes=["w2b0"], dma_sem=dsem("w2b0"))
                    P.op("pool", DMA(w2b[:, 4:8, :], w2v[:, 4:8, :]), writes=["w2b1"], dma_sem=dsem("w2b1"))

                for idx in range(3):
                    issue_w1(idx)
                issue_w2(0)
                for e_i in range(NE):
                    for j in range(8):
                        idx = e_i * 8 + j
                        ws = idx % 4
                        if idx + 3 < NE * 8:
                            issue_w1(idx + 3)
                        for tg in range(4):
                            tsl = slice(tg * 512, (tg + 1) * 512)
                            i = sw[0] % 2
                            sw[0] += 1
                            bg, bu = 2 * i, 2 * i + 1
                            P.op("pe", MM(pb[bg][:, :], [(w1b[ws][:, k, 0:128], hfT[:, k, tsl]) for k in range(8)]),
                                 reads=[("w1b", ws), "hfT"], writes=[PK(bg)])
                            P.op("pe", MM(pb[bu][:, :], [(w1b[ws][:, k, 128:256], hfT[:, k, tsl]) for k in range(8)]),
                                 reads=[("w1b", ws), "hfT"], writes=[PK(bu)])
                            c_g = e_i * 16 + j
                            c_u = e_i * 16 + 8 + j
                            P.op("dve", TS(gc[i][:], pb[bg][:, :], b1c[:, c_g:c_g + 1], 7.0, ALU.add, ALU.min), reads=[PK(bg)], writes=[("gc", i)])
                            P.op("act", ACT(sg[i][:], gc[i][:], AF.Sigmoid, scale=1.702), reads=[("gc", i)], writes=[("sg", i)])
                            P.op("dve", TS(uc[i][:], pb[bu][:, :], b1c[:, c_u:c_u + 1], 7.0, ALU.add, ALU.min), reads=[PK(bu)], writes=[("uc", i)])
                            P.op("dve", TS(uc[i][:], uc[i][:], -7.0, 1.0, ALU.max, ALU.add), reads=[("uc", i)], writes=[("uc", i)])
                            P.op("pool", TT(gc[i][:], gc[i][:], sg[i][:], ALU.mult), reads=[("gc", i), ("sg", i)], writes=[("gc", i)])
                            P.op("pool", TT(actT[:, j, tsl], gc[i][:], uc[i][:], ALU.mult), reads=[("gc", i), ("uc", i)], writes=[("actT", tg, j)])
                    for tl in range(16):
                        T = hf_i * 16 + tl
                        tk = slice(tl * 128, (tl + 1) * 128)
                        for hlf in range(2):
                            bk = 4 + (yb[0] % 4)
                            yb[0] += 1
                            cs = slice(hlf * 512, (hlf + 1) * 512)
                            pairs = [(actT[:, j, tk], w2b[:, j, cs]) for j in range(8)]
                            pairs.append((onehot[:, e_i * 128:(e_i + 1) * 128], b2b[:, cs]))
                            P.op("pe", MM(pb[bk][:, :], pairs), reads=[("actT", tl // 4, j) for j in range(8)] + ["w2b0", "w2b1"], writes=[PK(bk)])
                            if e_i == 0:
                                P.op("dve", TS(acc2[:, tl, cs], pb[bk][:, :], G[:, T, e_i:e_i + 1], None, ALU.mult),
                                     reads=[PK(bk)], writes=[("acc2", tl, hlf)])
                            else:
                                P.op("dve", STT(acc2[:, tl, cs], pb[bk][:, :], G[:, T, e_i:e_i + 1], acc2[:, tl, cs], ALU.mult, ALU.add),
                                     reads=[PK(bk), ("acc2", tl, hlf)], writes=[("acc2", tl, hlf)])
                    if e_i + 1 < NE:
                        issue_w2(e_i + 1)
                P.barrier()
                for tl in range(16):
                    T = hf_i * 16 + tl
                    s = tl % 2
                    tk = slice(T * 128, (T + 1) * 128)
                    P.op("sync", DMA(x5[s][:], XN[tk, :]), writes=[("x5", s)], dma_sem=dsem("x5_%d" % s))
                    P.op("dve", TT(o5[s][:], acc2[:, tl, :], gf_bc[:], ALU.mult), reads=[], writes=[("o5", s)])
                    P.op("pool", TT(x5[s][:], x5[s][:], o5[s][:], ALU.add), reads=[("x5", s), ("o5", s)], writes=[("x5", s)])
                    kk = "s5_%d" % s
                    P.op("act", ACT(o5[s][:], x5[s][:], AF.Square, accum_out=st5[:, 4 * s:4 * s + 1]), reads=[("x5", s)], writes=[("o5", s), kk + "_ss"])
                    rstd_chain(st5[:, 4 * s:4 * s + 1], None, st5[:, 4 * s + 1:4 * s + 2], st5[:, 4 * s + 2:4 * s + 3], 1024.0, kk)
                    P.op("dve", STT(o5[s][:], x5[s][:], st5[:, 4 * s + 2:4 * s + 3], fnw_bc, ALU.mult, ALU.mult),
                         reads=[("x5", s), kk + "_r", ("o5", s)], writes=[("o5", s)])
                    P.op("sync", DMA(out_d[tk, :], o5[s][:]), reads=[("o5", s)], writes=["out"], dma_sem=dsem("o5_%d" % s))
                P.barrier()
        if stop_phase < 4:
            P.barrier()

        P.finalize()
        with nc.Block() as block:
            @block.sync
            def _(e):
                P.emit("sync", e)

            @block.scalar
            def _(e):
                P.emit("act", e)

            @block.vector
            def _(e):
                P.emit("dve", e)

            @block.gpsimd
            def _(e):
                P.emit("pool", e)

            @block.tensor
            def _(e):
                P.emit("pe", e)
    return nc


def _rope_tables():
    n = 8192
    P_ = 16
    pos = np.arange(n)
    row = (pos // 64).astype(np.float32)
    col = (pos % 64).astype(np.float32)
    inv = (np.float32(10000.0) ** (-np.arange(P_, dtype=np.float32) / np.float32(P_))).astype(np.float32)
    ang_r = (row[:, None] * inv[None, :]).astype(np.float32)
    ang_c = (col[:, None] * inv[None, :]).astype(np.float32)
    C = np.zeros((64, n), np.float32)
    S = np.zeros((64, n), np.float32)
    for d in range(64):
        axis = d // 32
        half = (d % 32) // 16
        p = d % 16
        ang = (ang_r if axis == 0 else ang_c)[:, p].astype(np.float64)
        C[d] = np.cos(ang)
        S[d] = (-np.sin(ang)) if half == 0 else np.sin(ang)
    return np.concatenate([C, C], 0), np.concatenate([S, S], 0)


def _partner64():
    p = np.arange(64)
    return np.where((p % 32) < 16, p + 16, p - 16)


def prep_inputs(inp):
    f = lambda a: np.ascontiguousarray(np.asarray(a, dtype=np.float32))
    x, c, ctx, c_ctx = f(inp["x"]), f(inp["c"]), f(inp["ctx"]), f(inp["c_ctx"])
    w_in = f(inp["w_in"])[0]
    part = _partner64()
    dq, dk, dv = w_in[:, 0:512], w_in[:, 512:1024], w_in[:, 1024:1536]
    cq, ckv, kr = w_in[:, 1536:1792], w_in[:, 1792:1920], w_in[:, 1920:1984]
    perm512 = (np.arange(512) // 64) * 64 + part[np.arange(512) % 64]
    w_in_x = np.ascontiguousarray(np.concatenate([dq, dq[:, perm512], dk, dk[:, perm512], dv, cq, ckv, kr, kr[:, part]], axis=1))
    assert w_in_x.shape[1] == WCOLS
    w_uq = f(inp["w_uq"])[0].reshape(256, 4, 192)
    nope = w_uq[:, :, 0:128].reshape(256, 512)
    rope = w_uq[:, :, 128:192].reshape(256, 256)
    perm256 = (np.arange(256) // 64) * 64 + part[np.arange(256) % 64]
    w_uq_x = np.ascontiguousarray(np.concatenate([nope, rope, rope[:, perm256]], axis=1))
    w_ukv = f(inp["w_ukv"])[0].reshape(128, 4, 256)
    w_ukv_x = np.ascontiguousarray(np.concatenate([w_ukv[:, :, 0:128].reshape(128, 512), w_ukv[:, :, 128:256].reshape(128, 512)], axis=1))
    colT = lambda v: np.asarray(v, np.float32).reshape(-1, 128).T
    cols = np.concatenate([colT(inp["attn_norm_w"][0]), colT(inp["ffn_norm_w"][0]), colT(inp["b_ada"][0]),
                           colT(inp["q_norm_w"][0]), colT(inp["kv_norm_w"][0]), colT(inp["subln_w"][0])], axis=1)
    cols = np.ascontiguousarray(cols, dtype=np.float32)
    assert cols.shape == (128, 68)
    brow1 = np.concatenate([f(inp["final_norm_w"]), f(inp["router_b"])[0], f(inp["lambda_q1"])[0], f(inp["lambda_k1"])[0],
                            f(inp["lambda_q2"])[0], f(inp["lambda_k2"])[0]])
    brow = np.ascontiguousarray(np.tile(brow1[None, :], (128, 1)))
    b1c = np.ascontiguousarray(f(inp["b1"])[0].reshape(32, 16, 128).transpose(2, 0, 1).reshape(128, 512))
    onehot = np.zeros((32, 32, 128), np.float32)
    for e in range(32):
        onehot[e, e, :] = 1.0
    onehot = onehot.reshape(32, 4096)
    w1a = np.ascontiguousarray(f(inp["w1"])[0].reshape(32, 8, 128, 2, 8, 128).transpose(0, 4, 2, 1, 3, 5).reshape(32, 8, 128, 8, 256))
    w2 = f(inp["w2"])[0]
    kcst = np.zeros((128, 256), np.float32)
    kcst[:, 0:128] = np.triu(np.ones((128, 128), np.float32), 1)
    kcst[0:32, 128:160] = np.triu(np.ones((32, 32), np.float32), 0)
    kcst[:, 160:168] = (np.arange(8) * BR)[None, :]
    kcst[:, 168:168 + NB] = (np.arange(NB) * BR)[None, :]
    kcst[:, 232:240] = np.arange(8)[None, :] * 128 + np.arange(128)[:, None]
    kcst[:, 240] = np.arange(128)
    b1rows = np.ascontiguousarray(f(inp["b1"])[0].reshape(32 * 16, 128))
    ropeC_nat, ropeS_nat = _rope_tables()
    shared = {
        "w_ada": f(inp["w_ada"])[0], "cols": cols, "brow": brow, "ident": np.eye(128, dtype=np.float32), "b1c": b1c,
        "onehot": onehot, "b2": f(inp["b2"])[0], "w_in_x": w_in_x, "w_uq_x": w_uq_x, "w_ukv_x": w_ukv_x,
        "w_out": f(inp["w_out"])[0], "router_w": f(inp["router_w"])[0], "w1a": w1a, "w2": w2, "kc": kcst, "b1": b1rows,
    }
    maps = []
    for core in range(8):
        b, qh = core // 2, core % 2
        own = slice(qh * 4096, (qh + 1) * 4096)
        oth = slice((1 - qh) * 4096, (2 - qh) * 4096)
        cvec = np.zeros((128, 16), np.float32)
        cvec[:, 0::2] = c[b].reshape(8, 128).T
        cvec[:, 1::2] = c_ctx.reshape(8, 128).T
        m = dict(shared)
        m.update({
            "xo": np.ascontiguousarray(x[b, own]), "xk": np.ascontiguousarray(x[b, oth]), "ctx": np.ascontiguousarray(ctx[b]),
            "cvec": cvec,
            "ropeC": np.ascontiguousarray(np.concatenate([ropeC_nat[:, own], ropeC_nat[:, oth]], axis=1)),
            "ropeS": np.ascontiguousarray(np.concatenate([ropeS_nat[:, own], ropeS_nat[:, oth]], axis=1)),
        })
        maps.append(m)
    return maps


_NC_CACHE = {}


def kernel(**inputs):
    maps = prep_inputs(inputs)
    if "nc" not in _NC_CACHE:
        _NC_CACHE["nc"] = build_nc()
    nc = _NC_CACHE["nc"]
    res = run_bass_kernel_spmd(nc, maps, core_ids=list(range(8)))
    out = np.zeros((4, 8192, 1024), np.float32)
    for core in range(8):
        b, qh = core // 2, core % 2
        out[b, qh * 4096:(qh + 1) * 4096] = np.asarray(res.results[core]["out"], dtype=np.float32)
    return out
```

```python
import os
import contextlib
import numpy as np
import concourse.bass as bass
import concourse.mybir as mybir
from concourse.bass_utils import run_bass_kernel_spmd

F32 = mybir.dt.float32
BF16 = mybir.dt.bfloat16
ALU = mybir.AluOpType
AF = mybir.ActivationFunctionType

NTOK = 4096
NKEY = 8448
NKT = 66
EPS = 1e-6
DIFF_SCALE = 64 ** -0.5
MLA_SCALE = 192 ** -0.5
LAM_INIT = 0.2
NE = 32
WCOLS = 3072
SPARSE = True
NB = 64
BR = 512
I32 = mybir.dt.int32


class Op:
    __slots__ = ("eng", "fn", "sem", "inc", "waits", "signals", "val")

    def __init__(self, eng, fn, sem, inc):
        self.eng, self.fn, self.sem, self.inc = eng, fn, sem, inc
        self.waits = []
        self.signals = inc == 16
        self.val = None


class Prog:
    def __init__(self, sem_alloc):
        self.ops = []
        self.sem_alloc = sem_alloc
        self.eng_sem = {}
        self.last_w = {}
        self.readers = {}
        self.phase_id = 0
        self.tail = {}

    def _engsem(self, eng):
        if eng not in self.eng_sem:
            self.eng_sem[eng] = self.sem_alloc("p%d_%s" % (self.phase_id, eng))
        return self.eng_sem[eng]

    def op(self, eng, fn, reads=(), writes=(), dma_sem=None):
        if dma_sem is not None:
            o = Op(eng, fn, dma_sem, 16)
            self.tail[id(dma_sem)] = o
        else:
            o = Op(eng, fn, self._engsem(eng), 1)
            self.tail[eng] = o
        deps = []
        for k in reads:
            w = self.last_w.get(k)
            if w is not None:
                deps.append(w)
        for k in writes:
            w = self.last_w.get(k)
            if w is not None:
                deps.append(w)
            deps.extend(self.readers.get(k, ()))
        seen = set()
        for d in deps:
            if d is o or id(d) in seen:
                continue
            seen.add(id(d))
            if d.eng == "pe" and eng == "pe" and d.inc == 1 and dma_sem is None:
                continue
            d.signals = True
            o.waits.append(d)
        for k in reads:
            self.readers.setdefault(k, []).append(o)
        for k in writes:
            self.last_w[k] = o
            self.readers[k] = []
        self.ops.append(o)
        return o

    def barrier(self):
        tails = list(self.tail.values())
        for t in tails:
            t.signals = True
        for eng in ("sync", "act", "dve", "pool", "pe"):
            o = Op(eng, None, None, 0)
            o.signals = False
            o.waits = list(tails)
            self.ops.append(o)
        self.tail = {}
        self.last_w = {}
        self.readers = {}
        self.phase_id += 1
        self.eng_sem = {}

    def finalize(self):
        counts = {}
        for o in self.ops:
            if o.signals:
                key = id(o.sem)
                counts[key] = counts.get(key, 0) + o.inc
                o.val = counts[key]

    def emit(self, name, e):
        waited = {}
        for o in self.ops:
            if o.eng != name:
                continue
            for d in o.waits:
                key = id(d.sem)
                if waited.get(key, 0) >= d.val:
                    continue
                waited[key] = d.val
                e.wait_ge(d.sem, d.val)
            if o.fn is None:
                continue
            ins = o.fn(e)
            if o.signals:
                ins.then_inc(o.sem, o.inc)


def MM(out, pairs):
    def fn(e):
        n = len(pairs)
        ins = None
        for i, (l, r) in enumerate(pairs):
            ins = e.matmul(out, lhsT=l, rhs=r, start=(i == 0), stop=(i == n - 1))
        return ins
    return fn


def MM1(out, l, r, start, stop):
    return lambda e: e.matmul(out, lhsT=l, rhs=r, start=start, stop=stop)


def TRS(out, ins_list, ident):
    def fn(e):
        ins = None
        for o, i in zip(out, ins_list):
            ins = e.transpose(out=o, in_=i, identity=ident)
        return ins
    return fn


def ACT(out, in_, func, **kw):
    return lambda e: e.activation(out=out, in_=in_, func=func, **kw)


def TS(out, in0, s1, s2, op0, op1=None):
    if op1 is None:
        return lambda e: e.tensor_scalar(out=out, in0=in0, scalar1=s1, scalar2=None, op0=op0)
    return lambda e: e.tensor_scalar(out=out, in0=in0, scalar1=s1, scalar2=s2, op0=op0, op1=op1)


def TT(out, in0, in1, op):
    return lambda e: e.tensor_tensor(out=out, in0=in0, in1=in1, op=op)


def STT(out, in0, scalar, in1, op0, op1):
    return lambda e: e.scalar_tensor_tensor(out=out, in0=in0, scalar=scalar, in1=in1, op0=op0, op1=op1)


def CP(out, in_):
    return lambda e: e.tensor_copy(out=out, in_=in_)


def RCP(out, in_):
    return lambda e: e.reciprocal(out=out, in_=in_)


def DMA(out, in_):
    return lambda e: e.dma_start(out=out, in_=in_)


def MEMSET(ap, v):
    return lambda e: e.memset(ap, v)


def build_nc(stop_phase=99, debug=False):
    nc = bass.Bass("TRN2", target_bir_lowering=False)

    def din(name, shape, dt=F32):
        return nc.dram_tensor(name, list(shape), dt, kind="ExternalInput").ap()

    def dscr(name, shape, dt):
        return nc.dram_tensor(name, list(shape), dt, kind=("ExternalOutput" if debug else "Internal")).ap()

    xo = din("xo", [NTOK, 1024])
    xk = din("xk", [NTOK, 1024])
    ctx_d = din("ctx", [256, 1024])
    cvec_d = din("cvec", [128, 16])
    wada_d = din("w_ada", [1024, 6144])
    cols_d = din("cols", [128, 68])
    brow_d = din("brow", [128, 1024 + 32 + 256])
    ident_d = din("ident", [128, 128])
    b1c_d = din("b1c", [128, 512])
    onehot_d = din("onehot", [32, 4096])
    b2_d = din("b2", [32, 1024])
    winx_d = din("w_in_x", [1024, WCOLS])
    wuq_d = din("w_uq_x", [256, 1024])
    wukv_d = din("w_ukv_x", [128, 1024])
    wout_d = din("w_out", [1024, 1024])
    rw_d = din("router_w", [1024, 32])
    if stop_phase >= 4:
        w1_d = din("w1a", [NE, 8, 128, 8, 256])
        w2_d = din("w2", [NE, 1024, 1024])
    kc_d = din("kc", [128, 256])
    b1_d = din("b1", [NE * 16, 128])
    HTOK = dscr("HTOK", [NTOK, 1024], BF16)
    XS = nc.dram_tensor("XS", [NB * BR, 1024], BF16, kind="Internal").ap()
    YS = nc.dram_tensor("YS", [NB * BR, 1024], BF16, kind="Internal").ap()
    ropec_d = din("ropeC", [128, 8192])
    ropes_d = din("ropeS", [128, 8192])
    out_d = nc.dram_tensor("out", [NTOK, 1024], F32, kind="ExternalOutput").ap()

    QD = dscr("QD", [4, 128, NTOK], BF16)
    KD = dscr("KD", [4, 128, NKEY], BF16)
    VD = dscr("VD", [4, 128, NKT, 128], BF16)
    QN = dscr("QN", [4, 128, NTOK], BF16)
    QR = dscr("QR", [4, 64, NTOK], BF16)
    KN = dscr("KN", [4, 128, NKEY], BF16)
    KR = dscr("KR", [64, NKEY], BF16)
    VM = dscr("VM", [4, 128, NKT, 128], BF16)
    XN = dscr("XN", [NTOK, 1024], F32)
    HF = dscr("HF", [8, 128, NTOK], BF16)
    dbg_mod = dscr("dbg_mod", [128, 96], F32) if debug else None
    dbg_mix = dscr("dbg_mix", [8, 128, NTOK], BF16) if debug else None
    dbg_G = dscr("dbg_G", [128, 32 * 32], F32) if debug else None
    if debug:
        dbg_dest = dscr("dbg_dest", [128, 128], F32)
        dbg_gate = dscr("dbg_gate", [128, 128], F32)
        dbg_blk = dscr("dbg_blk", [128, NB], F32)
        dbg_md = dscr("dbg_md", [32, 8], F32)
        dbg_xs = dscr("dbg_xs", [1024, 1024], BF16)
        dbg_ys = dscr("dbg_ys", [1024, 1024], F32)

    es = contextlib.ExitStack()
    with es:
        sems = []

        def sem_alloc(name):
            s = es.enter_context(nc.semaphore(name))
            sems.append(s)
            return s

        P = Prog(sem_alloc)
        dsem_pool = []
        dsem_map = {}

        def dsem(name):
            if name not in dsem_map:
                i = len(dsem_map)
                if i >= len(dsem_pool):
                    dsem_pool.append(sem_alloc("dq%d" % i))
                dsem_map[name] = dsem_pool[i]
            return dsem_map[name]

        _orig_barrier = P.barrier

        def _barrier():
            _orig_barrier()
            dsem_map.clear()
        P.barrier = _barrier

        SB_LO, SB_HI = 16512, 229344
        cur = [SB_LO]
        names = [0]
        limit = [SB_HI]
        LATE_BASE = SB_HI - 16384

        def sb(shape, dt):
            nbytes = int(np.prod(shape[1:])) * (2 if dt == BF16 else 4)
            off = (cur[0] + 63) // 64 * 64
            assert off + nbytes <= limit[0], ("SBUF overflow", off, nbytes)
            cur[0] = off + nbytes
            names[0] += 1
            return nc.alloc_sbuf_tensor_at("t%d" % names[0], list(shape), dt, offset=off)

        pb = [nc.alloc_psum_tensor("pb%d" % i, [128, 512], F32) for i in range(8)]
        pbb = [p.bitcast(BF16) for p in pb]

        def PK(i):
            return ("ps", i)

        ident32 = sb([128, 128], F32)
        identb = sb([128, 128], BF16)
        ones32 = sb([128, 128], F32)
        onesb = sb([128, 128], BF16)
        cols = sb([128, 68], F32)
        brow = sb([128, 1312], F32)
        modT = sb([128, 48, 2], F32)
        gw = sb([128, 8], F32)
        gcw = sb([128, 8], F32)
        gfw = sb([128, 8], F32)
        gf_bc = sb([128, 1024], F32)
        b1c = sb([128, 512], F32)
        G = sb([128, 32, 32], F32)
        onehot = sb([32, 4096], BF16)
        b2b = sb([32, 1024], BF16)
        lamc = sb([128, 8], F32)
        subw = sb([128, 1], F32)
        epsc = sb([128, 1], F32)
        kc = sb([128, 256], F32)
        PERSIST_END = cur[0]
        cur[0] = LATE_BASE
        Mall = sb([128, 32, 32], F32)
        LG = sb([128, 32, 32], F32)
        TOP = sb([128, 32, 8], F32)
        DESTI = sb([128, 4, 32], I32)
        GATE = sb([128, 4, 32], F32)
        WIDXI = sb([128, NB, 8], I32)
        B1IDXI = sb([16, NB], I32)
        B2IDXI = sb([2, NB], I32)
        assert cur[0] <= SB_HI
        cur[0] = PERSIST_END
        wuqb = sb([128, 2, 1024], BF16)
        wukvb = sb([128, 1024], BF16)

        sa_col = lambda k: modT[:, 0 + k, 0:1]
        csa_col = lambda k: modT[:, 0 + k, 1:2]
        sf_col = lambda k: modT[:, 24 + k, 0:1]
        fnw_bc = brow[:, 0:1024]
        rb_bc = brow[:, 1024:1056]

        cv = sb([128, 16], F32)
        scv = sb([128, 16], F32)
        sgv = sb([128, 16], F32)
        wad = [sb([128, 8, 1024], F32) for _ in range(2)]
        lt = sb([128, 128], F32)
        diag = sb([128, 128], F32)
        wuq32 = sb([128, 2, 1024], F32)
        wukv32 = sb([128, 1024], F32)
        P.op("sync", DMA(wuq32[:], wuq_d.rearrange("(k p) n -> p k n", p=128)), writes=["wuq32"], dma_sem=dsem("wuq32"))
        P.op("sync", DMA(wukv32[:], wukv_d), writes=["wukv32"], dma_sem=dsem("wukv32"))

        P.op("sync", DMA(cv[:], cvec_d), writes=["cv"], dma_sem=dsem("cv"))
        P.op("sync", DMA(cols[:], cols_d), writes=["cols"], dma_sem=dsem("cols"))
        P.op("sync", DMA(brow[:], brow_d), writes=["brow"], dma_sem=dsem("brow"))
        P.op("sync", DMA(ident32[:], ident_d), writes=["ident32"], dma_sem=dsem("ident"))
        P.op("sync", DMA(b1c[:], b1c_d), writes=["b1c"], dma_sem=dsem("b1c"))
        P.op("sync", DMA(kc[:], kc_d), writes=["kc"], dma_sem=dsem("kc"))
        P.op("pool", DMA(onehot[:], onehot_d), writes=["onehot"], dma_sem=dsem("onehot"))
        P.op("pool", DMA(b2b[:], b2_d), writes=["b2b"], dma_sem=dsem("b2b"))
        P.op("dve", CP(identb[:], ident32[:]), reads=["ident32"], writes=["identb"])
        P.op("dve", MEMSET(ones32[:], 1.0), writes=["ones32"])
        P.op("dve", MEMSET(onesb[:], 1.0), writes=["onesb"])
        P.op("dve", MEMSET(epsc[:], EPS), writes=["epsc"])
        P.op("act", ACT(sgv[:], cv[:], AF.Sigmoid), reads=["cv"], writes=["sgv"])
        P.op("dve", TT(scv[:], cv[:], sgv[:], ALU.mult), reads=["cv", "sgv"], writes=["scv"])
        for m in range(6):
            s = m % 2
            src = wada_d[:, m * 1024:(m + 1) * 1024].rearrange("(k p) n -> p k n", p=128)
            P.op("sync", DMA(wad[s][:, 0:4, :], src[:, 0:4, :]), writes=[("wad", s, 0)], dma_sem=dsem("wad%d_0" % s))
            P.op("sync", DMA(wad[s][:, 4:8, :], src[:, 4:8, :]), writes=[("wad", s, 1)], dma_sem=dsem("wad%d_1" % s))
            bank = m % 2
            for fc in range(8):
                pairs = [(wad[s][:, k, fc * 128:(fc + 1) * 128], scv[:, 2 * k:2 * k + 2]) for k in range(8)]
                P.op("pe", MM(pb[bank][:, 2 * fc:2 * fc + 2], pairs),
                     reads=[("wad", s, 0), ("wad", s, 1), "scv"], writes=[PK(bank)])
            pv = pb[bank][:, 0:16].rearrange("p (a b) -> p a b", b=2)
            for j in range(2):
                P.op("dve", TT(modT[:, m * 8:(m + 1) * 8, j], pv[:, :, j], cols[:, 16 + m * 8:16 + (m + 1) * 8], ALU.add),
                     reads=[PK(bank), "cols"], writes=["modT"])
        P.op("dve", STT(gw[:], modT[:, 8:16, 0], 1.0, cols[:, 0:8], ALU.add, ALU.mult), reads=["modT", "cols"], writes=["gw"])
        P.op("dve", STT(gcw[:], modT[:, 8:16, 1], 1.0, cols[:, 0:8], ALU.add, ALU.mult), reads=["modT", "cols"], writes=["gcw"])
        P.op("dve", STT(gfw[:], modT[:, 32:40, 0], 1.0, cols[:, 8:16], ALU.add, ALU.mult), reads=["modT", "cols"], writes=["gfw"])
        def bcast_gate(mi, dst, nm, colfn=None):
            for k in range(8):
                bank = 2 + (k % 2)
                col = modT[:, mi * 8 + k, 0:1] if colfn is None else colfn(k)
                P.op("dve", TS(diag[:], ident32[:], col, None, ALU.mult),
                     reads=["ident32", "modT"], writes=["diag"])
                P.op("pe", MM(pb[bank][:, 0:128], [(ones32[:], diag[:])]), reads=["ones32", "diag"], writes=[PK(bank)])
                P.op("act", ACT(dst[:, k * 128:(k + 1) * 128], pb[bank][:, 0:128], AF.Identity), reads=[PK(bank)], writes=[nm])
        bcast_gate(5, gf_bc, "gf_bc")
        lv = brow[:, 1056:1312]
        P.op("dve", TT(lt[:, 0:64], lv[:, 0:64], lv[:, 64:128], ALU.mult), reads=["brow"], writes=["lt"])
        P.op("dve", TT(lt[:, 64:128], lv[:, 128:192], lv[:, 192:256], ALU.mult), reads=["brow"], writes=["lt"])
        P.op("dve", lambda e: e.reduce_sum(out=lamc[:, 0:1], in_=lt[:, 0:64], axis=mybir.AxisListType.X), reads=["lt"], writes=["lamc"])
        P.op("dve", lambda e: e.reduce_sum(out=lamc[:, 1:2], in_=lt[:, 64:128], axis=mybir.AxisListType.X), reads=["lt"], writes=["lamc"])
        P.op("act", ACT(lamc[:, 2:4], lamc[:, 0:2], AF.Exp), reads=["lamc"], writes=["lamc"])
        P.op("dve", TT(lamc[:, 4:5], lamc[:, 3:4], lamc[:, 2:3], ALU.subtract), reads=["lamc"], writes=["lamc"])
        P.op("dve", TS(lamc[:, 4:5], lamc[:, 4:5], -LAM_INIT, None, ALU.add), reads=["lamc"], writes=["lamc"])
        P.op("dve", TS(subw[:], cols[:, 67:68], 1.0 - LAM_INIT, None, ALU.mult), reads=["cols"], writes=["subw"])
        for k in range(2):
            P.op("dve", TS(wuqb[:, k, :], wuq32[:, k, :], cols[:, 64 + k:65 + k], None, ALU.mult),
                 reads=["wuq32", "cols"], writes=["wuqb"])
        P.op("dve", TS(wukvb[:], wukv32[:], cols[:, 66:67], None, ALU.mult), reads=["wukv32", "cols"], writes=["wukvb"])
        if debug:
            P.op("sync", DMA(dbg_mod, modT[:].rearrange("p a b -> p (a b)")), reads=["modT"], writes=["dbg_mod"], dma_sem=dsem("dbg_mod"))
        P.barrier()

        def rstd_chain(ss_ap, ms_ap, sq_ap, r_ap, n, key, eng_extra_reads=()):
            P.op("act", ACT(sq_ap, ss_ap, AF.Sqrt, scale=1.0 / n, bias=epsc[0:ss_ap.shape[0], 0:1]),
                 reads=[key + "_ss", "epsc"] + list(eng_extra_reads), writes=[key + "_sq"])
            P.op("dve", RCP(r_ap, sq_ap), reads=[key + "_sq"], writes=[key + "_r"])

        if stop_phase >= 1:
            cur[0] = PERSIST_END
            wuqb = sb([128, 2, 1024], BF16)
            wukvb = sb([128, 1024], BF16)
            Wb = sb([128, 8, WCOLS], BF16)
            xt = [sb([128, 1024], F32) for _ in range(3)]
            junk = sb([128, 1024], BF16)
            xsb = [sb([128, 1024], BF16) for _ in range(2)]
            st = sb([128, 16], F32)
            hT = [sb([128, 8, 512], BF16) for _ in range(2)]
            rC = [sb([128, 512], F32) for _ in range(2)]
            rS = [sb([128, 512], F32) for _ in range(2)]
            t1 = [sb([128, 512], F32) for _ in range(2)]
            t2 = [sb([128, 512], F32) for _ in range(2)]
            t3 = [sb([128, 512], F32)] * 2
            cqT = sb([128, 2, 512], BF16)
            ckvT = sb([128, 512], BF16)
            sq32 = sb([128, 2, 512], F32)
            sqk32 = sb([128, 512], F32)
            rq_sq = sb([128, 512], F32)
            rq_bc = sb([128, 512], F32)
            rk_sq = sb([128, 512], F32)
            rk_bc = sb([128, 512], F32)
            rkc = sb([128, 12], F32)
            o_qd = [sb([128, 4, 512], BF16)] * 2
            o_kd = [sb([128, 4, 512], BF16) for _ in range(2)]
            o_vd = [sb([128, 4, 512], BF16) for _ in range(2)]
            o_qn = [sb([128, 4, 512], BF16)] * 2
            o_qr = [sb([128, 2, 512], BF16)] * 2
            o_kn = [sb([128, 4, 512], BF16) for _ in range(2)]
            o_kr = [sb([64, 512], BF16) for _ in range(2)]
            o_vm = [sb([128, 4, 512], BF16) for _ in range(2)]

            wv = winx_d.rearrange("(k p) n -> p k n", p=128)
            for i in range(4):
                P.op("pool", DMA(Wb[:, 2 * i:2 * i + 2, :], wv[:, 2 * i:2 * i + 2, :]), writes=[("Wb", i)], dma_sem=dsem("Wb%d" % i))
            WbK = [("Wb", i) for i in range(4)]

            groups = [("ctx", 0, 2)] + [("own", g * 512, 4) for g in range(8)] + [("oth", g * 512, 4) for g in range(8)]
            bank_rr = [0]

            def nbank():
                b = 2 + (bank_rr[0] % 6)
                bank_rr[0] += 1
                return b

            tile_ctr = [0]
            def tiles_gen(gi):
                (kind, T0, nt) = groups[gi]
                N = nt * 128
                s = gi % 2
                src = {"ctx": ctx_d, "own": xo, "oth": xk}[kind]
                key0 = {"ctx": 0, "own": 256, "oth": 256 + 4096}[kind] + T0
                rope = kind != "ctx"
                own = kind == "own"
                g_w = gcw if kind == "ctx" else gw
                sac = csa_col if kind == "ctx" else sa_col
                if rope:
                    rcol = T0 + (4096 if kind == "oth" else 0)
                    P.op("sync", DMA(rC[s][:], ropec_d[:, rcol:rcol + 512]), writes=[("rC", s)], dma_sem=dsem("rC%d" % s))
                    P.op("sync", DMA(rS[s][:], ropes_d[:, rcol:rcol + 512]), writes=[("rS", s)], dma_sem=dsem("rS%d" % s))
                for t in range(nt):
                    tc = tile_ctr[0]
                    tile_ctr[0] += 1
                    xs_ = tc % 3
                    ss_ = tc % 4
                    x2 = tc % 2
                    P.op("sync", DMA(xt[xs_][:], src[T0 + t * 128:T0 + (t + 1) * 128, :]), writes=[("xt", xs_)], dma_sem=dsem("xt%d" % xs_))
                    kk = "st%d" % ss_
                    P.op("act", ACT(junk[:], xt[xs_][:], AF.Square, accum_out=st[:, 4 * ss_:4 * ss_ + 1]),
                         reads=[("xt", xs_)], writes=["junk", kk + "_ss"])
                    rstd_chain(st[:, 4 * ss_:4 * ss_ + 1], None, st[:, 4 * ss_ + 1:4 * ss_ + 2], st[:, 4 * ss_ + 2:4 * ss_ + 3], 1024.0, kk)
                    P.op("dve", TS(xsb[x2][:], xt[xs_][:], st[:, 4 * ss_ + 2:4 * ss_ + 3], None, ALU.mult),
                         reads=[("xt", xs_), kk + "_r"], writes=[("xsb", x2)])
                    yield
                    tb = tc % 2
                    P.op("pe", TRS([pbb[tb][:, k * 128:(k + 1) * 128] for k in range(8)],
                                   [xsb[x2][:, k * 128:(k + 1) * 128] for k in range(8)], identb[:]),
                         reads=[("xsb", x2), "identb"], writes=[PK(tb)])
                    for k in range(8):
                        P.op("act", ACT(hT[s][:, k, t * 128:(t + 1) * 128], pbb[tb][:, k * 128:(k + 1) * 128], AF.Identity,
                                        scale=g_w[:, k:k + 1], bias=sac(k)),
                             reads=[PK(tb), "gw", "gcw", "modT"], writes=[("hT", s, t)])
                    yield

            def proj_gen(gi):
                (kind, T0, nt) = groups[gi]
                N = nt * 128
                s = gi % 2
                src = {"ctx": ctx_d, "own": xo, "oth": xk}[kind]
                key0 = {"ctx": 0, "own": 256, "oth": 256 + 4096}[kind] + T0
                rope = kind != "ctx"
                own = kind == "own"
                g_w = gcw if kind == "ctx" else gw
                sac = csa_col if kind == "ctx" else sa_col
                hTk = [("hT", s, t) for t in range(nt)]

                def proj(col0, width=128):
                    b = nbank()
                    pairs = [(Wb[:, k, col0:col0 + width], hT[s][:, k, 0:N]) for k in range(8)]
                    P.op("pe", MM(pb[b][0:width, 0:N], pairs), reads=hTk + WbK, writes=[PK(b)])
                    return b

                def rope_out(bA, bB, dst, rows, dkey, post_scale=None):
                    i = bank_rr[0] % 2
                    P.op("dve", TT(t1[i][0:rows, 0:N], pb[bA][0:rows, 0:N], rC[s][0:rows, 0:N], ALU.mult),
                         reads=[PK(bA), ("rC", s)], writes=[("t1", i)])
                    P.op("dve", TT(t2[i][0:rows, 0:N], pb[bB][0:rows, 0:N], rS[s][0:rows, 0:N], ALU.mult),
                         reads=[PK(bB), ("rS", s)], writes=[("t2", i)])
                    if post_scale is None:
                        P.op("dve", TT(dst, t1[i][0:rows, 0:N], t2[i][0:rows, 0:N], ALU.add),
                             reads=[("t1", i), ("t2", i)], writes=[dkey])
                    else:
                        P.op("dve", TT(t3[i][0:rows, 0:N], t1[i][0:rows, 0:N], t2[i][0:rows, 0:N], ALU.add),
                             reads=[("t1", i), ("t2", i)], writes=[("t3", 0)])
                        P.op("dve", TT(dst, t3[i][0:rows, 0:N], post_scale[0:rows, 0:N], ALU.mult),
                             reads=[("t3", 0), "rq_r"], writes=[dkey])

                if own:
                    for h in range(4):
                        bA = proj(h * 128)
                        bB = proj(512 + h * 128)
                        rope_out(bA, bB, o_qd[s][:, h, 0:N], 128, ("o_qd", 0))
                        yield
                    P.op("sync", DMA(QD[:, :, T0:T0 + N].rearrange("h p t -> p h t"), o_qd[s][:, :, 0:N]),
                         reads=[("o_qd", 0)], writes=["QD"], dma_sem=dsem("o_qd"))
                for h in range(4):
                    bA = proj(1024 + h * 128)
                    if rope:
                        bB = proj(1536 + h * 128)
                        rope_out(bA, bB, o_kd[s][:, h, 0:N], 128, ("o_kd", s))
                        yield
                    else:
                        P.op("act", ACT(o_kd[s][:, h, 0:N], pb[bA][:, 0:N], AF.Identity), reads=[PK(bA)], writes=[("o_kd", s)])
                        yield
                P.op("sync", DMA(KD[:, :, key0:key0 + N].rearrange("h p t -> p h t"), o_kd[s][:, :, 0:N]),
                     reads=[("o_kd", s)], writes=["KD"], dma_sem=dsem("o_kd%d" % s))
                bA = proj(2944, 64)
                if rope:
                    bB = proj(3008, 64)
                    rope_out(bA, bB, o_kr[s][:, 0:N], 64, ("o_kr", s))
                    yield
                else:
                    P.op("act", ACT(o_kr[s][:, 0:N], pb[bA][0:64, 0:N], AF.Identity), reads=[PK(bA)], writes=[("o_kr", s)])
                    yield
                P.op("sync", DMA(KR[:, key0:key0 + N], o_kr[s][:, 0:N]), reads=[("o_kr", s)], writes=["KR"], dma_sem=dsem("o_kr%d" % s))
                for t in range(nt):
                    b = nbank()
                    pairs = [(hT[s][:, k, t * 128:(t + 1) * 128], Wb[:, k, 2048:2560]) for k in range(8)]
                    P.op("pe", MM(pb[b][:, :], pairs), reads=[("hT", s, t)] + WbK, writes=[PK(b)])
                    P.op("act", ACT(o_vd[s][:, t, :], pb[b][:, :], AF.Identity), reads=[PK(b)], writes=[("o_vd", s)])
                    yield
                kt0 = key0 // 128
                for h in range(4):
                    P.op("sync", DMA(VD[h, :, kt0:kt0 + nt, :], o_vd[s][:, 0:nt, h * 128:(h + 1) * 128]),
                         reads=[("o_vd", s)], writes=["VD"], dma_sem=dsem("o_vd%d_%d" % (s, h)))
                b = proj(2816)
                P.op("act", ACT(ckvT[:, 0:N], pb[b][:, 0:N], AF.Identity), reads=[PK(b)], writes=["ckvT"])
                P.op("act", ACT(sqk32[:, 0:N], pb[b][:, 0:N], AF.Square), reads=[PK(b)], writes=["sqk32"])
                b = nbank()
                P.op("pe", MM(pb[b][:, 0:N], [(ones32[:], sqk32[:, 0:N])]), reads=["ones32", "sqk32"], writes=[PK(b), "rk_ss"])
                rstd_chain(pb[b][:, 0:N], None, rk_sq[:, 0:N], rk_bc[:, 0:N], 128.0, "rk", eng_extra_reads=[PK(b)])
                b = nbank()
                for t in range(nt):
                    P.op("pe", MM(pb[b][:, t:t + 1], [(sqk32[:, t * 128:(t + 1) * 128], ones32[:, 0:1])]),
                         reads=["sqk32", "ones32"], writes=[PK(b), "rkc_ss"])
                rstd_chain(pb[b][:, 0:nt], None, rkc[:, 0:nt], rkc[:, 4:4 + nt], 128.0, "rkc", eng_extra_reads=[PK(b)])
                for h in range(4):
                    b = nbank()
                    P.op("pe", MM(pb[b][:, 0:N], [(wukvb[:, h * 128:(h + 1) * 128], ckvT[:, 0:N])]), reads=["wukvb", "ckvT"], writes=[PK(b)])
                    P.op("dve", TT(o_kn[s][:, h, 0:N], pb[b][:, 0:N], rk_bc[:, 0:N], ALU.mult), reads=[PK(b), "rk_r"], writes=[("o_kn", s)])
                    yield
                P.op("sync", DMA(KN[:, :, key0:key0 + N].rearrange("h p t -> p h t"), o_kn[s][:, :, 0:N]),
                     reads=[("o_kn", s)], writes=["KN"], dma_sem=dsem("o_kn%d" % s))
                for t in range(nt):
                    b = nbank()
                    P.op("pe", MM(pb[b][:, :], [(ckvT[:, t * 128:(t + 1) * 128], wukvb[:, 512:1024])]), reads=["wukvb", "ckvT"], writes=[PK(b)])
                    P.op("act", ACT(o_vm[s][:, t, :], pb[b][:, :], AF.Identity, scale=rkc[:, 4 + t:5 + t]), reads=[PK(b), "rkc_r"], writes=[("o_vm", s)])
                    yield
                for h in range(4):
                    P.op("sync", DMA(VM[h, :, kt0:kt0 + nt, :], o_vm[s][:, 0:nt, h * 128:(h + 1) * 128]),
                         reads=[("o_vm", s)], writes=["VM"], dma_sem=dsem("o_vm%d_%d" % (s, h)))
                if own:
                    for c in range(2):
                        b = proj(2560 + c * 128)
                        P.op("act", ACT(cqT[:, c, :], pb[b][:, 0:N], AF.Identity), reads=[PK(b)], writes=["cqT"])
                        P.op("act", ACT(sq32[:, c, :], pb[b][:, 0:N], AF.Square), reads=[PK(b)], writes=["sq32"])
                    b = nbank()
                    P.op("pe", MM(pb[b][:, 0:N], [(ones32[:], sq32[:, 0, :]), (ones32[:], sq32[:, 1, :])]),
                         reads=["ones32", "sq32"], writes=[PK(b), "rq_ss"])
                    rstd_chain(pb[b][:, 0:N], None, rq_sq[:, 0:N], rq_bc[:, 0:N], 256.0, "rq", eng_extra_reads=[PK(b)])
                    for h in range(4):
                        b = nbank()
                        P.op("pe", MM(pb[b][:, 0:N], [(wuqb[:, k, h * 128:(h + 1) * 128], cqT[:, k, :]) for k in range(2)]),
                             reads=["wuqb", "cqT"], writes=[PK(b)])
                        P.op("dve", TT(o_qn[s][:, h, 0:N], pb[b][:, 0:N], rq_bc[:, 0:N], ALU.mult), reads=[PK(b), "rq_r"], writes=[("o_qn", 0)])
                        yield
                    P.op("sync", DMA(QN[:, :, T0:T0 + N].rearrange("h p t -> p h t"), o_qn[s][:, :, 0:N]),
                         reads=[("o_qn", 0)], writes=["QN"], dma_sem=dsem("o_qn"))
                    for r in range(2):
                        bA = nbank()
                        P.op("pe", MM(pb[bA][:, 0:N], [(wuqb[:, k, 512 + r * 128:512 + (r + 1) * 128], cqT[:, k, :]) for k in range(2)]),
                             reads=["wuqb", "cqT"], writes=[PK(bA)])
                        bB = nbank()
                        P.op("pe", MM(pb[bB][:, 0:N], [(wuqb[:, k, 768 + r * 128:768 + (r + 1) * 128], cqT[:, k, :]) for k in range(2)]),
                             reads=["wuqb", "cqT"], writes=[PK(bB)])
                        rope_out(bA, bB, o_qr[s][:, r, 0:N], 128, ("o_qr", 0), post_scale=rq_bc)
                        yield
                    for r in range(2):
                        for hh in range(2):
                            P.op("sync", DMA(QR[2 * r + hh, :, T0:T0 + N], o_qr[s][hh * 64:(hh + 1) * 64, r, 0:N]),
                                 reads=[("o_qr", 0)], writes=["QR"], dma_sem=dsem("o_qr_%d" % (2 * r + hh)))

            for _ in tiles_gen(0):
                pass
            for gi in range(len(groups)):
                tg = tiles_gen(gi + 1) if gi + 1 < len(groups) else iter(())
                step = 0
                for _ in proj_gen(gi):
                    step += 1
                    if step % 2 == 0:
                        next(tg, None)
                for _ in tg:
                    pass
            P.barrier()

        cur[0] = PERSIST_END
        mixT = sb([128, 8, NTOK], BF16)
        PH2_END = cur[0]
        if stop_phase >= 2:
            Kt = [sb([128, NKEY], BF16) for _ in range(2)]
            Vt = [sb([128, NKT, 128], BF16) for _ in range(2)]
            KRt = sb([128, NKEY], BF16)
            Qa = [sb([128, 512], BF16) for _ in range(2)]
            Qb = [sb([128, 512], BF16) for _ in range(2)]
            Pt = [sb([128, 512], BF16) for _ in range(6)]
            e0 = sb([128, 512], F32)
            e1 = sb([128, 512], F32)
            e2 = sb([128, 512], F32)
            e3 = sb([128, 512], F32)
            e4 = sb([128, 512], F32)
            e5 = sb([128, 512], F32)
            P.op("sync", DMA(KRt[0:64, :], KR), writes=["KRt"], dma_sem=dsem("KRt"))
            P.op("dve", MEMSET(KRt[64:128, :], 0.0), writes=["KRtz"])
            for i in range(2):
                P.op("dve", MEMSET(Qa[i][:], 0.0), writes=[("Qa", i)])
                P.op("dve", MEMSET(Qb[i][:], 0.0), writes=[("Qb", i)])
            pctr = [0]
            qctr = [0]

            def load_head(hh):
                s = hh % 2
                if hh < 4:
                    ksrc, vsrc = KD[hh], VD[hh]
                else:
                    ksrc, vsrc = KN[hh - 4], VM[hh - 4]
                P.op("sync", DMA(Kt[s][:, 0:4224], ksrc[:, 0:4224]), writes=[("Kt", s, 0)], dma_sem=dsem("Kt%d_0" % s))
                P.op("sync", DMA(Kt[s][:, 4224:NKEY], ksrc[:, 4224:NKEY]), writes=[("Kt", s, 1)], dma_sem=dsem("Kt%d_1" % s))
                P.op("sync", DMA(Vt[s][:, 0:33, :], vsrc[:, 0:33, :]), writes=[("Vt", s, 0)], dma_sem=dsem("Vt%d_0" % s))
                P.op("sync", DMA(Vt[s][:, 33:NKT, :], vsrc[:, 33:NKT, :]), writes=[("Vt", s, 1)], dma_sem=dsem("Vt%d_1" % s))

            racc = sb([128, 512], F32)
            racc1 = sb([128, 512], F32)
            LA = 3
            items = [(hh, qg, kt, c) for hh in range(8) for qg in range(8) for kt in range(NKT) for c in range(2 if hh < 4 else 1)]
            NI = len(items)

            def emit_S(idx):
                hh, qg, kt, c = items[idx]
                s = hh % 2
                diff = hh < 4
                h = hh if diff else hh - 4
                qs = (hh * 8 + qg) % 2
                q0 = qg * 512
                if kt == 0 and c == 0:
                    if diff:
                        P.op("sync", DMA(Qa[qs][0:64, :], QD[h, 0:64, q0:q0 + 512]), writes=[("Qa", qs)], dma_sem=dsem("Qa%d" % qs))
                        P.op("sync", DMA(Qb[qs][64:128, :], QD[h, 64:128, q0:q0 + 512]), writes=[("Qb", qs)], dma_sem=dsem("Qb%d" % qs))
                    else:
                        P.op("sync", DMA(Qa[qs][:, :], QN[h, :, q0:q0 + 512]), writes=[("Qa", qs)], dma_sem=dsem("Qa%d" % qs))
                        P.op("sync", DMA(Qb[qs][0:64, :], QR[h, :, q0:q0 + 512]), writes=[("Qb", qs)], dma_sem=dsem("Qb%d" % qs))
                half = 0 if kt < 33 else 1
                kcols = slice(kt * 128, (kt + 1) * 128)
                sbk = idx % 4
                if diff:
                    qsrc = Qa[qs] if c == 0 else Qb[qs]
                    P.op("pe", MM(pb[sbk][:, :], [(Kt[s][:, kcols], qsrc[:, :])]),
                         reads=[("Kt", s, half), ("Qa", qs), ("Qb", qs)], writes=[PK(sbk)])
                else:
                    P.op("pe", MM(pb[sbk][:, :], [(Kt[s][:, kcols], Qa[qs][:, :]), (KRt[:, kcols], Qb[qs][:, :])]),
                         reads=[("Kt", s, half), ("Qa", qs), ("Qb", qs), "KRt", "KRtz"], writes=[PK(sbk)])

            load_head(0)
            for idx in range(LA):
                emit_S(idx)
            for idx in range(NI):
                hh, qg, kt, c = items[idx]
                if qg == 0 and kt == 0 and c == 0 and hh + 1 < 8:
                    load_head(hh + 1)
                if idx + LA < NI:
                    emit_S(idx + LA)
                s = hh % 2
                diff = hh < 4
                h = hh if diff else hh - 4
                q0 = qg * 512
                half = 0 if kt < 33 else 1
                sbk = idx % 4
                pi = idx % 6
                scale = DIFF_SCALE if diff else MLA_SCALE
                P.op("act", ACT(Pt[pi][:], pb[sbk][:, :], AF.Exp, scale=scale), reads=[PK(sbk)], writes=[("Pt", pi)])
                P.op("pe", MM1(pb[4 + c][:, :], Vt[s][:, kt, :], Pt[pi][:], kt == 0, kt == NKT - 1),
                     reads=[("Vt", s, half), ("Pt", pi)], writes=[PK(4 + c)])
                if c == 1 and kt % 3 == 0:
                    if kt == 0:
                        P.op("dve", CP(racc1[:], Pt[pi][:]), reads=[("Pt", pi)], writes=["racc1"])
                    else:
                        P.op("dve", TT(racc1[:], racc1[:], Pt[pi][:], ALU.add), reads=[("Pt", pi), "racc1"], writes=["racc1"])
                elif c == 1:
                    P.op("pe", MM1(pb[7][:, :], onesb[:], Pt[pi][:], kt == 1, False),
                         reads=["onesb", ("Pt", pi)], writes=[PK(7)])
                elif (not diff) and kt % 3 == 2:
                    P.op("pe", MM1(pb[6][:, :], onesb[:], Pt[pi][:], kt == 2, False),
                         reads=["onesb", ("Pt", pi)], writes=[PK(6)])
                else:
                    if kt == 0:
                        P.op("dve", CP(racc[:], Pt[pi][:]), reads=[("Pt", pi)], writes=["racc"])
                    else:
                        P.op("dve", TT(racc[:], racc[:], Pt[pi][:], ALU.add), reads=[("Pt", pi), "racc"], writes=["racc"])
                last = (kt == NKT - 1) and (c == (1 if diff else 0))
                if not last:
                    continue
                P.op("pe", MM1(pb[6][:, :], ones32[:], racc[:], diff, True), reads=["ones32", "racc"], writes=[PK(6)])
                if diff:
                    P.op("pe", MM1(pb[7][:, :], ones32[:], racc1[:], False, True), reads=["ones32", "racc1"], writes=[PK(7)])
                if diff:
                    P.op("dve", RCP(e0[:], pb[6][:, :]), reads=[PK(6)], writes=["e0"])
                    P.op("dve", TT(e1[:], pb[4][:, :], e0[:], ALU.mult), reads=[PK(4), "e0"], writes=["e1"])
                    P.op("dve", RCP(e2[:], pb[7][:, :]), reads=[PK(7)], writes=["e2"])
                    P.op("dve", TT(e3[:], pb[5][:, :], e2[:], ALU.mult), reads=[PK(5), "e2"], writes=["e3"])
                    P.op("dve", STT(e1[:], e3[:], lamc[:, 4:5], e1[:], ALU.mult, ALU.add), reads=["e1", "e3", "lamc"], writes=["e1"])
                    P.op("act", ACT(e0[:], e1[:], AF.Square), reads=["e1"], writes=["e0"])
                    P.op("pe", MM(pb[6][:, :], [(ones32[:], e0[:])]), reads=["ones32", "e0"], writes=[PK(6), "sub_ss"])
                    rstd_chain(pb[6][:, :], None, e4[:], e5[:], 128.0, "sub", eng_extra_reads=[PK(6)])
                    P.op("dve", STT(mixT[:, h, q0:q0 + 512], e1[:], subw[:, 0:1], e5[:], ALU.mult, ALU.mult),
                         reads=["e1", "sub_r", "subw"], writes=[("mixT", hh, qg)])
                else:
                    P.op("dve", RCP(e0[:], pb[6][:, :]), reads=[PK(6)], writes=["e0"])
                    P.op("dve", TT(mixT[:, 4 + h, q0:q0 + 512], pb[4][:, :], e0[:], ALU.mult), reads=[PK(4), "e0"], writes=[("mixT", hh, qg)])
            if debug:
                P.op("sync", DMA(dbg_mix.rearrange("k p t -> p k t"), mixT[:]), reads=[("mixT", a, b_) for a in range(8) for b_ in range(8)],
                     writes=["dbg_mix"], dma_sem=dsem("dbg_mix"))
            P.barrier()

        if stop_phase >= 3:
            limit[0] = LATE_BASE
            cur[0] = PH2_END
            woutb = sb([128, 8, 1024], BF16)
            rw32 = sb([128, 8, 32], F32)
            x3 = [sb([128, 1024], F32) for _ in range(2)]
            y3 = [sb([128, 1024], F32) for _ in range(2)]
            xn3 = [sb([128, 1024], F32) for _ in range(2)]
            xs3 = [sb([128, 1024], F32) for _ in range(2)]
            hf32 = [sb([128, 8, 128], F32) for _ in range(2)]
            hfb = [sb([128, 8, 128], BF16) for _ in range(2)]
            st3 = sb([128, 8], F32)
            gfw_bc = sb([128, 1024], F32)
            sf_bc = sb([128, 1024], F32)
            htk = [sb([128, 1024], BF16) for _ in range(2)]
            ex = [sb([128, 32], F32) for _ in range(2)]
            gs3 = sb([128, 8], F32)
            ga_bc = sb([128, 1024], F32)
            diag = sb([128, 128], F32)
            bcast_gate(2, ga_bc, "ga_bc")
            bcast_gate(0, gfw_bc, "gfw_bc", colfn=lambda k: gfw[:, k:k + 1])
            bcast_gate(0, sf_bc, "sf_bc", colfn=sf_col)
            wo_v = wout_d.rearrange("(k p) n -> p k n", p=128)
            P.op("pool", DMA(woutb[:, 0:4, :], wo_v[:, 0:4, :]), writes=["woutb0"], dma_sem=dsem("woutb0"))
            P.op("pool", DMA(woutb[:, 4:8, :], wo_v[:, 4:8, :]), writes=["woutb1"], dma_sem=dsem("woutb1"))
            P.op("sync", DMA(rw32[:], rw_d.rearrange("(k p) n -> p k n", p=128)), writes=["rw32"], dma_sem=dsem("rw32"))
            for t in range(32):
                s = t % 2
                tk = slice(t * 128, (t + 1) * 128)
                P.op("sync", DMA(x3[s][:], xo[tk, :]), writes=[("x3", s)], dma_sem=dsem("x3_%d" % s))
                for hlf in range(2):
                    bk = hlf + 2 * s
                    P.op("pe", MM(pb[bk][:, :], [(mixT[:, k, tk], woutb[:, k, hlf * 512:(hlf + 1) * 512]) for k in range(8)]),
                         reads=["woutb0", "woutb1"], writes=[PK(bk)])
                    P.op("dve", TT(y3[s][:, hlf * 512:(hlf + 1) * 512], pb[bk][:, :], ga_bc[:, hlf * 512:(hlf + 1) * 512], ALU.mult),
                         reads=[PK(bk)], writes=[("y3", s)])
                P.op("dve", TT(xn3[s][:], y3[s][:], x3[s][:], ALU.add), reads=[("y3", s), ("x3", s)], writes=[("xn3", s)])
                P.op("sync", DMA(XN[tk, :], xn3[s][:]), reads=[("xn3", s)], writes=["XN"], dma_sem=dsem("xn3_%d" % s))
                kk = "s3_%d" % s
                P.op("act", ACT(y3[s][:], xn3[s][:], AF.Square, accum_out=st3[:, 4 * s:4 * s + 1]), reads=[("xn3", s)], writes=[("y3", s), kk + "_ss"])
                rstd_chain(st3[:, 4 * s:4 * s + 1], None, st3[:, 4 * s + 1:4 * s + 2], st3[:, 4 * s + 2:4 * s + 3], 1024.0, kk)
                P.op("dve", TS(xs3[s][:], xn3[s][:], st3[:, 4 * s + 2:4 * s + 3], None, ALU.mult), reads=[("xn3", s), kk + "_r"], writes=[("xs3", s)])
                for hb in range(2):
                    bk = 4 + 2 * s + hb
                    P.op("pe", TRS([pb[bk][:, j * 128:(j + 1) * 128] for j in range(4)],
                                   [xs3[s][:, (4 * hb + j) * 128:(4 * hb + j + 1) * 128] for j in range(4)], ident32[:]),
                         reads=[("xs3", s), "ident32"], writes=[PK(bk)])
                    for j in range(4):
                        k = 4 * hb + j
                        P.op("act", ACT(hf32[s][:, k, :], pb[bk][:, j * 128:(j + 1) * 128], AF.Identity, scale=gfw[:, k:k + 1], bias=sf_col(k)),
                             reads=[PK(bk)], writes=[("hf32", s)])
                if not SPARSE:
                    P.op("pool", CP(hfb[s][:], hf32[s][:]), reads=[("hf32", s)], writes=[("hfb", s)])
                    P.op("sync", DMA(HF[:, :, tk].rearrange("k p t -> p k t"), hfb[s][:]), reads=[("hfb", s)], writes=["HF"], dma_sem=dsem("hfb%d" % s))
                else:
                    P.op("dve", TT(xs3[s][:], xs3[s][:], gfw_bc[:], ALU.mult), reads=[("xs3", s), "gfw_bc"] + [PK(4 + 2 * s), PK(5 + 2 * s)], writes=[("xs3", s)])
                    P.op("dve", TT(htk[s][:], xs3[s][:], sf_bc[:], ALU.add), reads=[("xs3", s), "sf_bc"], writes=[("htk", s)])
                    P.op("sync", DMA(HTOK[tk, :], htk[s][:]), reads=[("htk", s)], writes=["HTOK"], dma_sem=dsem("htk%d" % s))
                bk = s
                lb = 4 + 2 * s
                P.op("pe", MM(pb[lb][:, 0:32], [(hf32[s][:, k, :], rw32[:, k, :]) for k in range(8)]),
                     reads=[("hf32", s), "rw32"], writes=[PK(lb)])
                lg_t, top_t, msk_t = LG[:, t, :], TOP[:, t, :], Mall[:, t, :]
                P.op("dve", TT(lg_t, pb[lb][:, 0:32], rb_bc, ALU.add), reads=[PK(lb)], writes=[("lg", s)])
                P.op("dve", (lambda o_, i_: (lambda e: e.max(out=o_, in_=i_)))(top_t, lg_t), reads=[("lg", s)], writes=[("top8", s)])
                P.op("dve", TS(msk_t, lg_t, TOP[:, t, 3:4], None, ALU.is_ge), reads=[("lg", s), ("top8", s)], writes=[("msk", s)])
                P.op("dve", TS(gs3[:, 4 * s:4 * s + 1], TOP[:, t, 0:1], -1.0, None, ALU.mult), reads=[("top8", s)], writes=[("gs3", s, 0)])
                P.op("act", ACT(ex[s][:], lg_t, AF.Exp, bias=gs3[:, 4 * s:4 * s + 1]), reads=[("lg", s), ("gs3", s, 0)], writes=[("ex", s)])
                P.op("dve", TT(ex[s][:], ex[s][:], msk_t, ALU.mult), reads=[("ex", s), ("msk", s)], writes=[("ex", s)])
                P.op("dve", lambda e, s=s: e.reduce_sum(out=gs3[:, 4 * s + 1:4 * s + 2], in_=ex[s][:], axis=mybir.AxisListType.X),
                     reads=[("ex", s)], writes=[("gs3", s, 1)])
                P.op("dve", RCP(gs3[:, 4 * s + 2:4 * s + 3], gs3[:, 4 * s + 1:4 * s + 2]), reads=[("gs3", s, 1)], writes=[("gs3", s, 2)])
                P.op("dve", TS(G[:, t, :], ex[s][:], gs3[:, 4 * s + 2:4 * s + 3], None, ALU.mult), reads=[("ex", s), ("gs3", s, 2)], writes=["G"])
            if debug:
                P.op("sync", DMA(dbg_G, G[:].rearrange("p a b -> p (a b)")), reads=["G"], writes=["dbg_G"], dma_sem=dsem("dbg_G"))
            P.barrier()

        def IGATHER(out, in_, idx):
            return lambda e: e.indirect_dma_start(out=out, out_offset=None, in_=in_, in_offset=bass.IndirectOffsetOnAxis(ap=idx, axis=0))

        def ISCATTER(out, idx, in_):
            return lambda e: e.indirect_dma_start(out=out, out_offset=bass.IndirectOffsetOnAxis(ap=idx, axis=0), in_=in_, in_offset=None)

        def RSUM(out, in_):
            return lambda e: e.reduce_sum(out=out, in_=in_, axis=mybir.AxisListType.X)

        if stop_phase >= 4 and SPARSE:
            cur[0] = PERSIST_END
            md = sb([32, 8], F32)
            cmp8 = sb([32, 8], F32)
            psbc = sb([32, 128], F32)
            base = sb([128, 32], F32)
            DEST = sb([128, 32, 32], F32)
            sel = sb([128, 32, 32], F32)
            tmp = sb([128, 32, 32], F32)
            DESTF = sb([128, 4, 32], F32)
            cmpj = sb([32, NB], F32)
            blkbc = sb([128, NB], F32)
            WIDXF = sb([128, NB, 8], F32)
            B1F = sb([16, NB], F32)
            htk4 = [sb([128, 1024], BF16) for _ in range(2)]
            P.op("pe", MM(pb[0][0:32, 0:1], [(Mall[:, t, :], ones32[:, 0:1]) for t in range(32)]), reads=[], writes=[PK(0)])
            P.op("dve", CP(md[:, 0:1], pb[0][0:32, 0:1]), reads=[PK(0)], writes=["md0"])
            P.op("dve", TS(cmp8[:], kc[0:32, 160:168], md[:, 0:1], None, ALU.is_lt), reads=["md0"], writes=["cmp8"])
            P.op("dve", RSUM(md[:, 1:2], cmp8[:]), reads=["cmp8"], writes=["md1"])
            P.op("dve", TS(md[:, 2:3], md[:, 1:2], float(BR), None, ALU.mult), reads=["md1"], writes=["md2"])
            P.op("pe", MM(pb[1][0:32, 0:1], [(kc[0:32, 128:160], md[:, 2:3])]), reads=["md2"], writes=[PK(1)])
            P.op("dve", CP(md[:, 3:4], pb[1][0:32, 0:1]), reads=[PK(1)], writes=["md3"])
            P.op("dve", TT(md[:, 4:5], md[:, 3:4], md[:, 2:3], ALU.subtract), reads=["md3", "md2"], writes=["md4"])
            P.op("dve", TS(psbc[:], ones32[0:32, :], md[:, 4:5], None, ALU.mult), reads=["md4"], writes=["psbc"])
            P.op("pe", MM(pb[2][:, 0:32], [(psbc[:], ident32[0:32, 0:32])]), reads=["psbc"], writes=[PK(2)])
            P.op("dve", CP(base[:], pb[2][:, 0:32]), reads=[PK(2)], writes=["base"])
            for i in range(32):
                bk, bk2 = 3 + i % 2, 5 + i % 2
                P.op("pe", MM(pb[bk][:, 0:32], [(kc[:, 0:128], Mall[:, i, :])]), reads=[], writes=[PK(bk)])
                P.op("pe", MM(pb[bk2][:, 0:32], [(ones32[:], Mall[:, i, :])]), reads=[], writes=[PK(bk2)])
                P.op("dve", TT(DEST[:, i, :], pb[bk][:, 0:32], base[:], ALU.add), reads=[PK(bk), "base"], writes=["DEST"])
                P.op("dve", TT(base[:], base[:], pb[bk2][:, 0:32], ALU.add), reads=[PK(bk2), "base"], writes=["base"])
            sel2 = [sb([128, 32], F32) for _ in range(2)]
            tmp2 = [sb([128, 32], F32) for _ in range(2)]
            tmp3 = [sb([128, 32], F32) for _ in range(2)]
            c3 = [0]
            for i in range(32):
                for k in range(4):
                    z = c3[0] % 2
                    c3[0] += 1
                    P.op("dve", TS(sel2[z][:], LG[:, i, :], TOP[:, i, k:k + 1], None, ALU.is_equal), reads=[], writes=[("sel2", z)])
                    P.op("dve", TT(tmp2[z][:], sel2[z][:], DEST[:, i, :], ALU.mult), reads=[("sel2", z), "DEST"], writes=[("tmp2", z)])
                    P.op("dve", RSUM(DESTF[:, k, i:i + 1], tmp2[z][:]), reads=[("tmp2", z)], writes=[("DESTF", k)])
                    P.op("dve", TT(tmp3[z][:], sel2[z][:], G[:, i, :], ALU.mult), reads=[("sel2", z)], writes=[("tmp3", z)])
                    P.op("dve", RSUM(GATE[:, k, i:i + 1], tmp3[z][:]), reads=[("tmp3", z)], writes=[("GATE", k)])
            P.op("dve", TS(DESTF[:], DESTF[:], 0.0, float(NB * BR - 1), ALU.max, ALU.min), reads=[("DESTF", k) for k in range(4)], writes=[("DESTF", k) for k in range(4)])
            P.op("dve", CP(DESTI[:], DESTF[:]), reads=[("DESTF", k) for k in range(4)], writes=["DESTI"])
            P.op("dve", TS(cmpj[:], kc[0:32, 168:168 + NB], md[:, 3:4], None, ALU.is_ge), reads=["md3"], writes=["cmpj"])
            P.op("pe", MM(pb[7][:, 0:NB], [(ones32[0:32, :], cmpj[:])]), reads=["cmpj"], writes=[PK(7)])
            P.op("dve", TS(blkbc[:], pb[7][:, 0:NB], float(NE - 1), None, ALU.min), reads=[PK(7)], writes=["blkbc"])
            for jj in range(8):
                P.op("dve", TS(WIDXF[:, :, jj], blkbc[:], 1024.0, kc[:, 232 + jj:233 + jj], ALU.mult, ALU.add), reads=["blkbc"], writes=["WIDXF"])
            P.op("dve", CP(WIDXI[:], WIDXF[:]), reads=["WIDXF"], writes=["WIDXI"])
            P.op("dve", TS(B1F[:], blkbc[0:16, :], 16.0, kc[0:16, 240:241], ALU.mult, ALU.add), reads=["blkbc"], writes=["B1F"])
            P.op("dve", CP(B1IDXI[:], B1F[:]), reads=["B1F"], writes=["B1IDXI"])
            P.op("dve", CP(B2IDXI[:], blkbc[0:2, :]), reads=["blkbc"], writes=["B2IDXI"])
            if debug:
                P.op("sync", DMA(dbg_dest, DESTF[:].rearrange("p a b -> p (a b)")), reads=[("DESTF", k) for k in range(4)], writes=["dbg_dest"], dma_sem=dsem("dbg1"))
                P.op("sync", DMA(dbg_gate, GATE[:].rearrange("p a b -> p (a b)")), reads=[("GATE", k) for k in range(4)], writes=["dbg_gate"], dma_sem=dsem("dbg2"))
                P.op("sync", DMA(dbg_blk, blkbc[:]), reads=["blkbc"], writes=["dbg_blk"], dma_sem=dsem("dbg3"))
                P.op("sync", DMA(dbg_md, md[:]), reads=["md0", "md1", "md2", "md3", "md4"], writes=["dbg_md"], dma_sem=dsem("dbg4"))
            for i in range(32):
                s = i % 2
                P.op("sync", DMA(htk4[s][:], HTOK[i * 128:(i + 1) * 128, :]), writes=[("htk4", s)], dma_sem=dsem("htk4_%d" % s))
                for k in range(4):
                    P.op("pool", ISCATTER(XS, DESTI[:, k, i:i + 1], htk4[s][:]), reads=[("htk4", s), "DESTI"], writes=[("XSw", s, k)],
                         dma_sem=dsem("sc%d_%d" % (s, k)))
            P.barrier()

            cur[0] = PERSIST_END
            w1b = [sb([128, 2048], BF16) for _ in range(16)]
            w2b = [sb([128, 8, 1024], BF16) for _ in range(2)]
            b1g = [sb([16, 128], F32) for _ in range(2)]
            b2r = [sb([2, 1024], BF16) for _ in range(2)]
            b1col = [sb([128, 16], F32) for _ in range(2)]
            xtok = [sb([128, 4, 1024], BF16) for _ in range(2)]
            xT = sb([128, 8, 512], BF16)
            actT = sb([128, 8, 512], BF16)
            gc = [sb([128, 512], F32) for _ in range(2)]
            sg = [sb([128, 512], F32) for _ in range(2)]
            uc = [sb([128, 512], F32) for _ in range(2)]
            ysb = [sb([128, 1024], BF16) for _ in range(2)]
            w1flat = w1_d.rearrange("e j p k c -> (e j p) (k c)")
            w2flat = w2_d.rearrange("e r n -> (e r) n")

            def issue_block_loads(b):
                ws = b % 2
                P.op("sync", DMA(xtok[ws][:], XS[b * BR:(b + 1) * BR, :].rearrange("(t p) d -> p t d", p=128)), writes=[("xtok", ws)], dma_sem=dsem("xtok%d" % ws))
                for jj in range(8):
                    slot = (b * 8 + jj) % 16
                    P.op("pool", IGATHER(w1b[slot][:], w1flat, WIDXI[:, b, jj:jj + 1]), writes=[("w1b", slot)], dma_sem=dsem("w1b%d" % slot))
                P.op("pool", IGATHER(b1g[ws][:], b1_d, B1IDXI[:, b:b + 1]), writes=[("b1g", ws)], dma_sem=dsem("b1g%d" % ws))
                for jj in range(8):
                    P.op("pool", IGATHER(w2b[ws][:, jj, :], w2flat, WIDXI[:, b, jj:jj + 1]), writes=[("w2b", ws, jj)], dma_sem=dsem("w2b%d_%d" % (ws, jj)))
                P.op("pool", IGATHER(b2r[ws][:], b2_d, B2IDXI[:, b:b + 1]), writes=[("b2r", ws)], dma_sem=dsem("b2r%d" % ws))

            issue_block_loads(0)
            sw = [0]
            yb = [0]
            ys_ctr = [0]
            for b in range(NB):
                ws = b % 2
                if b + 1 < NB:
                    issue_block_loads(b + 1)
                for t in range(4):
                    tb = t % 2
                    P.op("pe", TRS([pbb[tb][:, k * 128:(k + 1) * 128] for k in range(8)],
                                   [xtok[ws][:, t, k * 128:(k + 1) * 128] for k in range(8)], identb[:]),
                         reads=[("xtok", ws)], writes=[PK(tb)])
                    P.op("act", ACT(xT[:, :, t * 128:(t + 1) * 128], pbb[tb][:, :].rearrange("p (k t) -> p k t", k=8), AF.Identity),
                         reads=[PK(tb)], writes=[("xT", t)])
                P.op("pe", TRS([pb[0][:, 0:16]], [b1g[ws][:, :]], ident32[0:16, 0:16]), reads=[("b1g", ws)], writes=[PK(0)])
                P.op("dve", CP(b1col[ws][:], pb[0][:, 0:16]), reads=[PK(0)], writes=[("b1col", ws)])
                xTk = [("xT", t) for t in range(4)]
                for jj in range(8):
                    slot = (b * 8 + jj) % 16
                    i = sw[0] % 2
                    sw[0] += 1
                    bg, bu = 2 + 2 * i, 3 + 2 * i
                    P.op("pe", MM(pb[bg][:, :], [(w1b[slot][:, k * 256:k * 256 + 128], xT[:, k, :]) for k in range(8)]),
                         reads=[("w1b", slot)] + xTk, writes=[PK(bg)])
                    P.op("pe", MM(pb[bu][:, :], [(w1b[slot][:, k * 256 + 128:k * 256 + 256], xT[:, k, :]) for k in range(8)]),
                         reads=[("w1b", slot)] + xTk, writes=[PK(bu)])
                    P.op("dve", TS(gc[i][:], pb[bg][:, :], b1col[ws][:, jj:jj + 1], 7.0, ALU.add, ALU.min), reads=[PK(bg), ("b1col", ws)], writes=[("gc", i)])
                    P.op("act", ACT(sg[i][:], gc[i][:], AF.Sigmoid, scale=1.702), reads=[("gc", i)], writes=[("sg", i)])
                    P.op("dve", TS(uc[i][:], pb[bu][:, :], b1col[ws][:, 8 + jj:9 + jj], 7.0, ALU.add, ALU.min), reads=[PK(bu), ("b1col", ws)], writes=[("uc", i)])
                    P.op("dve", TS(uc[i][:], uc[i][:], -7.0, 1.0, ALU.max, ALU.add), reads=[("uc", i)], writes=[("uc", i)])
                    P.op("dve", TT(gc[i][:], gc[i][:], sg[i][:], ALU.mult), reads=[("gc", i), ("sg", i)], writes=[("gc", i)])
                    P.op("dve", TT(actT[:, jj, :], gc[i][:], uc[i][:], ALU.mult), reads=[("gc", i), ("uc", i)], writes=[("actT", jj)])
                for t in range(4):
                    ys_ = ys_ctr[0] % 2
                    ys_ctr[0] += 1
                    for hlf in range(2):
                        bk = 6 + (yb[0] % 2)
                        yb[0] += 1
                        cs = slice(hlf * 512, (hlf + 1) * 512)
                        pairs = [(actT[:, jj, t * 128:(t + 1) * 128], w2b[ws][:, jj, cs]) for jj in range(8)]
                        pairs.append((onesb[0:1, 0:128], b2r[ws][0:1, cs]))
                        P.op("pe", MM(pb[bk][:, :], pairs), reads=[("actT", jj) for jj in range(8)] + [("w2b", ws, jj) for jj in range(8)] + [("b2r", ws)],
                             writes=[PK(bk)])
                        P.op("act", ACT(ysb[ys_][:, cs], pb[bk][:, :], AF.Identity), reads=[PK(bk)], writes=[("ysb", ys_, hlf)])
                    P.op("sync", DMA(YS[b * BR + t * 128:b * BR + (t + 1) * 128, :], ysb[ys_][:]), reads=[("ysb", ys_, 0), ("ysb", ys_, 1)],
                         writes=["YS"], dma_sem=dsem("ysb%d" % ys_))
            P.barrier()

            if debug:
                P.op("sync", DMA(dbg_xs, XS[0:1024, :]), writes=["dbg_xs"], dma_sem=dsem("dbg5"))
                P.op("sync", DMA(dbg_ys, YS[0:1024, :]), writes=["dbg_ys"], dma_sem=dsem("dbg6"))
                P.barrier()
            cur[0] = PERSIST_END
            yk = [[sb([128, 1024], BF16) for _ in range(4)] for _ in range(2)]
            acc5 = [sb([128, 1024], F32) for _ in range(2)]
            x5 = [sb([128, 1024], F32) for _ in range(2)]
            o5 = [sb([128, 1024], F32) for _ in range(2)]
            st5 = sb([128, 8], F32)

            def issue_gathers(i):
                s = i % 2
                for k in range(4):
                    P.op("pool", IGATHER(yk[s][k][:], YS, DESTI[:, k, i:i + 1]), writes=[("yk", s, k)], dma_sem=dsem("yk%d_%d" % (s, k)))
                P.op("sync", DMA(x5[s][:], XN[i * 128:(i + 1) * 128, :]), writes=[("x5", s)], dma_sem=dsem("x5_%d" % s))

            issue_gathers(0)
            for i in range(32):
                s = i % 2
                if i + 1 < 32:
                    issue_gathers(i + 1)
                P.op("dve", TS(acc5[s][:], yk[s][0][:], GATE[:, 0, i:i + 1], None, ALU.mult), reads=[("yk", s, 0)], writes=[("acc5", s)])
                for k in range(1, 4):
                    P.op("dve", STT(acc5[s][:], yk[s][k][:], GATE[:, k, i:i + 1], acc5[s][:], ALU.mult, ALU.add),
                         reads=[("yk", s, k), ("acc5", s)], writes=[("acc5", s)])
                P.op("dve", TT(o5[s][:], acc5[s][:], gf_bc[:], ALU.mult), reads=[("acc5", s)], writes=[("o5", s)])
                P.op("dve", TT(x5[s][:], x5[s][:], o5[s][:], ALU.add), reads=[("x5", s), ("o5", s)], writes=[("x5", s)])
                kk = "s5_%d" % s
                P.op("act", ACT(o5[s][:], x5[s][:], AF.Square, accum_out=st5[:, 4 * s:4 * s + 1]), reads=[("x5", s)], writes=[("o5", s), kk + "_ss"])
                rstd_chain(st5[:, 4 * s:4 * s + 1], None, st5[:, 4 * s + 1:4 * s + 2], st5[:, 4 * s + 2:4 * s + 3], 1024.0, kk)
                P.op("dve", STT(o5[s][:], x5[s][:], st5[:, 4 * s + 2:4 * s + 3], fnw_bc, ALU.mult, ALU.mult),
                     reads=[("x5", s), kk + "_r", ("o5", s)], writes=[("o5", s)])
                P.op("sync", DMA(out_d[i * 128:(i + 1) * 128, :], o5[s][:]), reads=[("o5", s)], writes=["out"], dma_sem=dsem("o5_%d" % s))
            P.barrier()

        if stop_phase >= 4 and not SPARSE:
            cur[0] = PERSIST_END
            acc2 = sb([128, 16, 1024], F32)
            hfT = sb([128, 8, 2048], BF16)
            off_act = (cur[0] + 63) // 64 * 64
            actT = sb([128, 8, 2048], BF16)
            w1b = [sb([128, 8, 256], BF16) for _ in range(4)]
            w2b = sb([128, 8, 1024], BF16)
            gc = [sb([128, 512], F32) for _ in range(2)]
            sg = [sb([128, 512], F32) for _ in range(2)]
            uc = [sb([128, 512], F32) for _ in range(2)]
            st5 = sb([128, 8], F32)
            end4 = cur[0]
            cur[0] = off_act
            x5 = [sb([128, 1024], F32) for _ in range(2)]
            o5 = [sb([128, 1024], F32) for _ in range(2)]
            cur[0] = end4
            sw = [0]
            yb = [0]
            for hf_i in range(2):
                tok0 = hf_i * 2048
                for k in range(8):
                    P.op("sync", DMA(hfT[:, k, :], HF[k, :, tok0:tok0 + 2048]), writes=["hfT"], dma_sem=dsem("hfT%d" % k))

                def issue_w1(idx):
                    ws_ = idx % 4
                    P.op("pool", DMA(w1b[ws_][:], w1_d[idx // 8, idx % 8]), writes=[("w1b", ws_)], dma_sem=dsem("w1b%d" % ws_))

                def issue_w2(e_):
                    w2v = w2_d[e_].rearrange("(j p) n -> p j n", p=128)
                    P.op("pool", DMA(w2b[:, 0:4, :], w2v[:, 0:4, :]), writes=["w2b0"], dma_sem=dsem("w2b0"))
                    P.op("pool", DMA(w2b[:, 4:8, :], w2v[:, 4:8, :]), writes=["w2b1"], dma_sem=dsem("w2b1"))

                for idx in range(3):
                    issue_w1(idx)
                issue_w2(0)
                for e_i in range(NE):
                    for j in range(8):
                        idx = e_i * 8 + j
                        ws = idx % 4
                        if idx + 3 < NE * 8:
                            issue_w1(idx + 3)
                        for tg in range(4):
                            tsl = slice(tg * 512, (tg + 1) * 512)
                            i = sw[0] % 2
                            sw[0] += 1
                            bg, bu = 2 * i, 2 * i + 1
                            P.op("pe", MM(pb[bg][:, :], [(w1b[ws][:, k, 0:128], hfT[:, k, tsl]) for k in range(8)]),
                                 reads=[("w1b", ws), "hfT"], writes=[PK(bg)])
                            P.op("pe", MM(pb[bu][:, :], [(w1b[ws][:, k, 128:256], hfT[:, k, tsl]) for k in range(8)]),
                                 reads=[("w1b", ws), "hfT"], writes=[PK(bu)])
                            c_g = e_i * 16 + j
                            c_u = e_i * 16 + 8 + j
                            P.op("dve", TS(gc[i][:], pb[bg][:, :], b1c[:, c_g:c_g + 1], 7.0, ALU.add, ALU.min), reads=[PK(bg)], writes=[("gc", i)])
                            P.op("act", ACT(sg[i][:], gc[i][:], AF.Sigmoid, scale=1.702), reads=[("gc", i)], writes=[("sg", i)])
                            P.op("dve", TS(uc[i][:], pb[bu][:, :], b1c[:, c_u:c_u + 1], 7.0, ALU.add, ALU.min), reads=[PK(bu)], writes=[("uc", i)])
                            P.op("dve", TS(uc[i][:], uc[i][:], -7.0, 1.0, ALU.max, ALU.add), reads=[("uc", i)], writes=[("uc", i)])
                            P.op("pool", TT(gc[i][:], gc[i][:], sg[i][:], ALU.mult), reads=[("gc", i), ("sg", i)], writes=[("gc", i)])
                            P.op("pool", TT(actT[:, j, tsl], gc[i][:], uc[i][:], ALU.mult), reads=[("gc", i), ("uc", i)], writes=[("actT", tg, j)])
                    for tl in range(16):
                        T = hf_i * 16 + tl
                        tk = slice(tl * 128, (tl + 1) * 128)
                        for hlf in range(2):
                            bk = 4 + (yb[0] % 4)
                            yb[0] += 1
                            cs = slice(hlf * 512, (hlf + 1) * 512)
                            pairs = [(actT[:, j, tk], w2b[:, j, cs]) for j in range(8)]
                            pairs.append((onehot[:, e_i * 128:(e_i + 1) * 128], b2b[:, cs]))
                            P.op("pe", MM(pb[bk][:, :], pairs), reads=[("actT", tl // 4, j) for j in range(8)] + ["w2b0", "w2b1"], writes=[PK(bk)])
                            if e_i == 0:
                                P.op("dve", TS(acc2[:, tl, cs], pb[bk][:, :], G[:, T, e_i:e_i + 1], None, ALU.mult),
                                     reads=[PK(bk)], writes=[("acc2", tl, hlf)])
                            else:
                                P.op("dve", STT(acc2[:, tl, cs], pb[bk][:, :], G[:, T, e_i:e_i + 1], acc2[:, tl, cs], ALU.mult, ALU.add),
                                     reads=[PK(bk), ("acc2", tl, hlf)], writes=[("acc2", tl, hlf)])
                    if e_i + 1 < NE:
                        issue_w2(e_i + 1)
                P.barrier()
                for tl in range(16):
                    T = hf_i * 16 + tl
                    s = tl % 2
                    tk = slice(T * 128, (T + 1) * 128)
                    P.op("sync", DMA(x5[s][:], XN[tk, :]), writes=[("x5", s)], dma_sem=dsem("x5_%d" % s))
                    P.op("dve", TT(o5[s][:], acc2[:, tl, :], gf_bc[:], ALU.mult), reads=[], writes=[("o5", s)])
                    P.op("pool", TT(x5[s][:], x5[s][:], o5[s][:], ALU.add), reads=[("x5", s), ("o5", s)], writes=[("x5", s)])
                    kk = "s5_%d" % s
                    P.op("act", ACT(o5[s][:], x5[s][:], AF.Square, accum_out=st5[:, 4 * s:4 * s + 1]), reads=[("x5", s)], writes=[("o5", s), kk + "_ss"])
                    rstd_chain(st5[:, 4 * s:4 * s + 1], None, st5[:, 4 * s + 1:4 * s + 2], st5[:, 4 * s + 2:4 * s + 3], 1024.0, kk)
                    P.op("dve", STT(o5[s][:], x5[s][:], st5[:, 4 * s + 2:4 * s + 3], fnw_bc, ALU.mult, ALU.mult),
                         reads=[("x5", s), kk + "_r", ("o5", s)], writes=[("o5", s)])
                    P.op("sync", DMA(out_d[tk, :], o5[s][:]), reads=[("o5", s)], writes=["out"], dma_sem=dsem("o5_%d" % s))
                P.barrier()
        if stop_phase < 4:
            P.barrier()

        P.finalize()
        with nc.Block() as block:
            @block.sync
            def _(e):
                P.emit("sync", e)

            @block.scalar
            def _(e):
                P.emit("act", e)

            @block.vector
            def _(e):
                P.emit("dve", e)

            @block.gpsimd
            def _(e):
                P.emit("pool", e)

            @block.tensor
            def _(e):
                P.emit("pe", e)
    return nc


def _rope_tables():
    n = 8192
    P_ = 16
    pos = np.arange(n)
    row = (pos // 64).astype(np.float32)
    col = (pos % 64).astype(np.float32)
    inv = (np.float32(10000.0) ** (-np.arange(P_, dtype=np.float32) / np.float32(P_))).astype(np.float32)
    ang_r = (row[:, None] * inv[None, :]).astype(np.float32)
    ang_c = (col[:, None] * inv[None, :]).astype(np.float32)
    C = np.zeros((64, n), np.float32)
    S = np.zeros((64, n), np.float32)
    for d in range(64):
        axis = d // 32
        half = (d % 32) // 16
        p = d % 16
        ang = (ang_r if axis == 0 else ang_c)[:, p].astype(np.float64)
        C[d] = np.cos(ang)
        S[d] = (-np.sin(ang)) if half == 0 else np.sin(ang)
    return np.concatenate([C, C], 0), np.concatenate([S, S], 0)


def _partner64():
    p = np.arange(64)
    return np.where((p % 32) < 16, p + 16, p - 16)


def prep_inputs(inp):
    f = lambda a: np.ascontiguousarray(np.asarray(a, dtype=np.float32))
    x, c, ctx, c_ctx = f(inp["x"]), f(inp["c"]), f(inp["ctx"]), f(inp["c_ctx"])
    w_in = f(inp["w_in"])[0]
    part = _partner64()
    dq, dk, dv = w_in[:, 0:512], w_in[:, 512:1024], w_in[:, 1024:1536]
    cq, ckv, kr = w_in[:, 1536:1792], w_in[:, 1792:1920], w_in[:, 1920:1984]
    perm512 = (np.arange(512) // 64) * 64 + part[np.arange(512) % 64]
    w_in_x = np.ascontiguousarray(np.concatenate([dq, dq[:, perm512], dk, dk[:, perm512], dv, cq, ckv, kr, kr[:, part]], axis=1))
    assert w_in_x.shape[1] == WCOLS
    w_uq = f(inp["w_uq"])[0].reshape(256, 4, 192)
    nope = w_uq[:, :, 0:128].reshape(256, 512)
    rope = w_uq[:, :, 128:192].reshape(256, 256)
    perm256 = (np.arange(256) // 64) * 64 + part[np.arange(256) % 64]
    w_uq_x = np.ascontiguousarray(np.concatenate([nope, rope, rope[:, perm256]], axis=1))
    w_ukv = f(inp["w_ukv"])[0].reshape(128, 4, 256)
    w_ukv_x = np.ascontiguousarray(np.concatenate([w_ukv[:, :, 0:128].reshape(128, 512), w_ukv[:, :, 128:256].reshape(128, 512)], axis=1))
    colT = lambda v: np.asarray(v, np.float32).reshape(-1, 128).T
    cols = np.concatenate([colT(inp["attn_norm_w"][0]), colT(inp["ffn_norm_w"][0]), colT(inp["b_ada"][0]),
                           colT(inp["q_norm_w"][0]), colT(inp["kv_norm_w"][0]), colT(inp["subln_w"][0])], axis=1)
    cols = np.ascontiguousarray(cols, dtype=np.float32)
    assert cols.shape == (128, 68)
    brow1 = np.concatenate([f(inp["final_norm_w"]), f(inp["router_b"])[0], f(inp["lambda_q1"])[0], f(inp["lambda_k1"])[0],
                            f(inp["lambda_q2"])[0], f(inp["lambda_k2"])[0]])
    brow = np.ascontiguousarray(np.tile(brow1[None, :], (128, 1)))
    b1c = np.ascontiguousarray(f(inp["b1"])[0].reshape(32, 16, 128).transpose(2, 0, 1).reshape(128, 512))
    onehot = np.zeros((32, 32, 128), np.float32)
    for e in range(32):
        onehot[e, e, :] = 1.0
    onehot = onehot.reshape(32, 4096)
    w1a = np.ascontiguousarray(f(inp["w1"])[0].reshape(32, 8, 128, 2, 8, 128).transpose(0, 4, 2, 1, 3, 5).reshape(32, 8, 128, 8, 256))
    w2 = f(inp["w2"])[0]
    kcst = np.zeros((128, 256), np.float32)
    kcst[:, 0:128] = np.triu(np.ones((128, 128), np.float32), 1)
    kcst[0:32, 128:160] = np.triu(np.ones((32, 32), np.float32), 0)
    kcst[:, 160:168] = (np.arange(8) * BR)[None, :]
    kcst[:, 168:168 + NB] = (np.arange(NB) * BR)[None, :]
    kcst[:, 232:240] = np.arange(8)[None, :] * 128 + np.arange(128)[:, None]
    kcst[:, 240] = np.arange(128)
    b1rows = np.ascontiguousarray(f(inp["b1"])[0].reshape(32 * 16, 128))
    ropeC_nat, ropeS_nat = _rope_tables()
    shared = {
        "w_ada": f(inp["w_ada"])[0], "cols": cols, "brow": brow, "ident": np.eye(128, dtype=np.float32), "b1c": b1c,
        "onehot": onehot, "b2": f(inp["b2"])[0], "w_in_x": w_in_x, "w_uq_x": w_uq_x, "w_ukv_x": w_ukv_x,
        "w_out": f(inp["w_out"])[0], "router_w": f(inp["router_w"])[0], "w1a": w1a, "w2": w2, "kc": kcst, "b1": b1rows,
    }
    maps = []
    for core in range(8):
        b, qh = core // 2, core % 2
        own = slice(qh * 4096, (qh + 1) * 4096)
        oth = slice((1 - qh) * 4096, (2 - qh) * 4096)
        cvec = np.zeros((128, 16), np.float32)
        cvec[:, 0::2] = c[b].reshape(8, 128).T
        cvec[:, 1::2] = c_ctx.reshape(8, 128).T
        m = dict(shared)
        m.update({
            "xo": np.ascontiguousarray(x[b, own]), "xk": np.ascontiguousarray(x[b, oth]), "ctx": np.ascontiguousarray(ctx[b]),
            "cvec": cvec,
            "ropeC": np.ascontiguousarray(np.concatenate([ropeC_nat[:, own], ropeC_nat[:, oth]], axis=1)),
            "ropeS": np.ascontiguousarray(np.concatenate([ropeS_nat[:, own], ropeS_nat[:, oth]], axis=1)),
        })
        maps.append(m)
    return maps


_NC_CACHE = {}


def kernel(**inputs):
    maps = prep_inputs(inputs)
    if "nc" not in _NC_CACHE:
        _NC_CACHE["nc"] = build_nc()
    nc = _NC_CACHE["nc"]
    res = run_bass_kernel_spmd(nc, maps, core_ids=list(range(8)))
    out = np.zeros((4, 8192, 1024), np.float32)
    for core in range(8):
        b, qh = core // 2, core % 2
        out[b, qh * 4096:(qh + 1) * 4096] = np.asarray(res.results[core]["out"], dtype=np.float32)
    return out
```
